# Optimizing a Trainium2 kernel written in Bass

```python
import jax, jax.numpy as jnp
from jax import lax
import numpy as np

D_MODEL = 1024
BATCH = 16
SEQ = 2048
DEPTH = 2

ATT_HEADS = 8
ATT_KV_HEADS = 2
ATT_HEAD_DIM = 64
ATT_GROUP = ATT_HEADS // ATT_KV_HEADS
WINDOW = 128
ATT_BLOCK = 128
RET_HEADS = 4
RET_KEY_DIM = 128
RET_VAL_DIM = 256
RET_CHUNK = 128
D_FF = -(-8 * D_MODEL // (3 * 256)) * 256
EPS = 1e-6

ATT_Q_W = ATT_HEADS * ATT_HEAD_DIM
ATT_KV_W = ATT_KV_HEADS * ATT_HEAD_DIM
RET_QK_W = RET_HEADS * RET_KEY_DIM
RET_V_W = RET_HEADS * RET_VAL_DIM
IN_WIDTHS = (ATT_Q_W, ATT_KV_W, ATT_KV_W, RET_QK_W, RET_QK_W, RET_V_W, RET_V_W, D_MODEL, D_MODEL)
D_IN = sum(IN_WIDTHS)

kernel_name = "hybrid_swa_sink_alibi_retention_gated_swiglu"


def rms_norm(x, g):
    xf = x.astype(jnp.float32)
    y = xf * lax.rsqrt(jnp.mean(xf * xf, axis=-1, keepdims=True) + EPS) * g.astype(jnp.float32)
    return y.astype(x.dtype)


def alibi_slopes(n_heads):
    return jnp.exp2(-8.0 * (jnp.arange(n_heads, dtype=jnp.float32) + 1.0) / n_heads)


def sliding_window_attention(q, k, v, sinks):
    b, s, _, hd = q.shape
    nb = s // ATT_BLOCK
    f32 = jnp.float32
    qb = q.astype(f32).reshape(b, nb, ATT_BLOCK, ATT_KV_HEADS, ATT_GROUP, hd) * (hd ** -0.5)

    def band(t):
        tp = jnp.pad(t.astype(f32), ((0, 0), (ATT_BLOCK, 0), (0, 0), (0, 0)))
        prev = tp[:, :s].reshape(b, nb, ATT_BLOCK, ATT_KV_HEADS, hd)
        cur = tp[:, ATT_BLOCK:].reshape(b, nb, ATT_BLOCK, ATT_KV_HEADS, hd)
        return jnp.concatenate([prev, cur], axis=2)

    kb, vb = band(k), band(v)
    i = jnp.arange(ATT_BLOCK)[:, None]
    j = jnp.arange(2 * ATT_BLOCK)[None, :]
    dist = i + ATT_BLOCK - j
    key_pos = jnp.arange(nb)[:, None, None] * ATT_BLOCK - ATT_BLOCK + j[None]
    valid = (dist >= 0) & (dist < WINDOW) & (key_pos >= 0)
    slopes = alibi_slopes(ATT_HEADS).reshape(ATT_KV_HEADS, ATT_GROUP)
    scores = jnp.einsum('bnikgd,bnjkd->bnkgij', qb, kb) - slopes[:, :, None, None] * dist.astype(f32)
    scores = jnp.where(valid[None, :, None, None], scores, -jnp.inf)
    sink = jnp.broadcast_to(sinks.astype(f32).reshape(ATT_KV_HEADS, ATT_GROUP)[None, None, :, :, None, None],
                            scores.shape[:-1] + (1,))
    probs = jax.nn.softmax(jnp.concatenate([scores, sink], axis=-1), axis=-1)[..., :-1]
    out = jnp.einsum('bnkgij,bnjkd->bnikgd', probs, vb)
    return out.reshape(b, s, ATT_Q_W)


def retention_chunkwise(q, k, v):
    b, s, h, dk = q.shape
    dv = v.shape[-1]
    nc = s // RET_CHUNK
    f32 = jnp.float32
    log_g = jnp.log(1.0 - jnp.exp2(-5.0 - jnp.arange(h, dtype=f32)))
    idx = jnp.arange(RET_CHUNK, dtype=f32)
    diff = idx[:, None] - idx[None, :]
    decay_intra = jnp.where(diff >= 0, jnp.exp(log_g[:, None, None] * jnp.maximum(diff, 0.0)), 0.0)
    decay_q = jnp.exp(log_g[:, None] * (idx + 1.0)).T[None, :, :, None]
    decay_k = jnp.exp(log_g[:, None] * (RET_CHUNK - 1.0 - idx)).T[None, :, :, None]
    decay_chunk = jnp.exp(log_g * RET_CHUNK)[None, :, None, None]

    def to_chunks(t):
        return t.astype(f32).reshape(b, nc, RET_CHUNK, h, t.shape[-1]).transpose(1, 0, 2, 3, 4)

    qc, kc, vc = to_chunks(q), to_chunks(k * (dk ** -0.5)), to_chunks(v)

    def step(state, inp):
        qi, ki, vi = inp
        scores = jnp.einsum('bihd,bjhd->bhij', qi, ki) * decay_intra
        o = jnp.einsum('bhij,bjhv->bihv', scores, vi)
        o = o + jnp.einsum('bihd,bhdv->bihv', qi, state) * decay_q
        state = decay_chunk * state + jnp.einsum('bjhd,bjhv->bhdv', ki * decay_k, vi)
        return state, o

    state0 = jnp.zeros((b, h, dk, dv), f32)
    _, o = lax.scan(step, state0, (qc, kc, vc))
    return o.transpose(1, 0, 2, 3, 4).reshape(b, s, h, dv)


def head_group_norm(o, gain):
    b, s, h, dv = o.shape
    mu = jnp.mean(o, axis=-1, keepdims=True)
    var = jnp.mean(jnp.square(o - mu), axis=-1, keepdims=True)
    return ((o - mu) * lax.rsqrt(var + EPS)).reshape(b, s, h * dv) * gain.astype(jnp.float32)


def setup_inputs(seed: int = 0) -> dict:
    key = jax.random.key(seed)
    ks = jax.random.split(key, 16)
    f32 = jnp.float32

    def w(k, shape, fan_in):
        return jax.random.normal(k, shape, f32) * (fan_in ** -0.5)

    return {
        "x": jax.random.normal(ks[0], (BATCH, SEQ, D_MODEL), f32),
        "norm_mix": 1.0 + 0.02 * jax.random.normal(ks[1], (DEPTH, D_MODEL), f32),
        "w_in": w(ks[2], (DEPTH, D_MODEL, D_IN), D_MODEL),
        "att_sinks": 0.5 * jax.random.normal(ks[3], (DEPTH, ATT_HEADS), f32),
        "ret_gn_gain": 1.0 + 0.02 * jax.random.normal(ks[4], (DEPTH, RET_V_W), f32),
        "w_att_o": w(ks[5], (DEPTH, ATT_Q_W, D_MODEL), ATT_Q_W),
        "w_ret_o": w(ks[6], (DEPTH, RET_V_W, D_MODEL), RET_V_W),
        "w_out": w(ks[7], (DEPTH, D_MODEL, D_MODEL), D_MODEL),
        "norm_ffn": 1.0 + 0.02 * jax.random.normal(ks[8], (DEPTH, D_MODEL), f32),
        "w_gate": w(ks[9], (DEPTH, D_MODEL, D_FF), D_MODEL),
        "w_up": w(ks[10], (DEPTH, D_MODEL, D_FF), D_MODEL),
        "w_down": w(ks[11], (DEPTH, D_FF, D_MODEL), D_FF),
        "final_norm": 1.0 + 0.02 * jax.random.normal(ks[12], (D_MODEL,), f32),
    }


def reference(x, norm_mix, w_in, att_sinks, ret_gn_gain, w_att_o, w_ret_o, w_out,
              norm_ffn, w_gate, w_up, w_down, final_norm):
    b, s, _ = x.shape
    offsets = [int(o) for o in np.cumsum(IN_WIDTHS)[:-1]]
    for l in range(DEPTH):
        h = rms_norm(x, norm_mix[l])
        proj = h @ w_in[l]
        aq, ak, av, rq, rk, rv, rg, ga, gr = jnp.split(proj, offsets, axis=-1)
        att = sliding_window_attention(
            aq.reshape(b, s, ATT_HEADS, ATT_HEAD_DIM),
            ak.reshape(b, s, ATT_KV_HEADS, ATT_HEAD_DIM),
            av.reshape(b, s, ATT_KV_HEADS, ATT_HEAD_DIM),
            att_sinks[l]).astype(x.dtype)
        ret = retention_chunkwise(
            rq.reshape(b, s, RET_HEADS, RET_KEY_DIM),
            rk.reshape(b, s, RET_HEADS, RET_KEY_DIM),
            rv.reshape(b, s, RET_HEADS, RET_VAL_DIM))
        ret = head_group_norm(ret, ret_gn_gain[l]).astype(x.dtype) * jax.nn.silu(rg)
        merged = jax.nn.sigmoid(ga) * (att @ w_att_o[l]) + jax.nn.sigmoid(gr) * (ret @ w_ret_o[l])
        x = x + merged @ w_out[l]
        h = rms_norm(x, norm_ffn[l])
        x = x + (jax.nn.silu(h @ w_gate[l]) * (h @ w_up[l])) @ w_down[l]
    return rms_norm(x, final_norm)
```

```python
from contextlib import ExitStack
import numpy as np
import concourse.bass as bass
import concourse.mybir as mybir
from concourse.bass_utils import run_bass_kernel_spmd

F32 = mybir.dt.float32
BF16 = mybir.dt.bfloat16
AF = mybir.ActivationFunctionType
ALU = mybir.AluOpType

ENGS = ("pe", "act", "dve", "pool", "sp")
SEM_CAP = 30000


class Op:
    __slots__ = ("eng", "fn", "idx", "waits", "signal", "tick", "slot", "count", "clock", "dclock", "tag")


class Prog:
    def __init__(self):
        self.streams = {e: [] for e in ENGS}
        self.last_w = {}
        self.readers = {}
        self.clock = {e: {} for e in ENGS}
        self.dclock = {e: {} for e in ENGS}
        self.slot_count = {}
        self.slot_last = {}
        self.out_dmas = []
        self.last_acc = {}
        self.tag = ""
        self.pe_log = []

    def add(self, eng, fn, reads=(), writes=(), slot=None, is_out=False):
        excl = [k for k in list(reads) + list(writes) if isinstance(k, tuple) and k[0] == "ps"]
        op = Op()
        op.eng, op.fn = eng, fn
        op.tag = self.tag
        st = self.streams[eng]
        op.idx = len(st)
        op.signal = False
        op.tick = None
        op.slot = slot
        op.waits = []
        deps = []
        seen = set()

        def push(d):
            if d is not None and id(d) not in seen:
                seen.add(id(d))
                deps.append(d)

        for k in reads:
            push(self.last_w.get(k))
        for k in writes:
            push(self.last_w.get(k))
            for r in self.readers.get(k, ()):
                push(r)
        if slot is not None:
            push(self.slot_last.get(slot))
        for k in excl:
            for e2, d in self.last_acc.setdefault(k, {}).items():
                if e2 != eng:
                    push(d)
        ck = self.clock[eng]
        dk = self.dclock[eng]
        for d in deps:
            if d.slot is not None:
                if dk.get(d.slot, 0) >= d.count:
                    continue
                op.waits.append(d)
                dk[d.slot] = d.count
            else:
                if d.eng == eng and eng == "pe":
                    continue
                if ck.get(d.eng, 0) >= d.idx + 1:
                    continue
                op.waits.append(d)
                d.signal = True
                ck[d.eng] = d.idx + 1
            for e2, v in d.clock.items():
                if ck.get(e2, 0) < v:
                    ck[e2] = v
            for s2, v in d.dclock.items():
                if dk.get(s2, 0) < v:
                    dk[s2] = v
        if slot is not None:
            c = self.slot_count.get(slot, 0) + 1
            self.slot_count[slot] = c
            op.count = c
            self.slot_last[slot] = op
            op.clock = {e: v for e, v in ck.items() if e != eng}
            op.dclock = dict(dk)
            if is_out:
                self.out_dmas.append(op)
        else:
            op.count = 0
            op.clock = dict(ck)
            op.dclock = dict(dk)
        for k in excl:
            self.last_acc[k][eng] = op
        for k in reads:
            self.readers.setdefault(k, []).append(op)
        for k in writes:
            self.last_w[k] = op
            self.readers[k] = []
        st.append(op)
        return op

    def emit(self, nc):
        nsig = {}
        for e in ENGS:
            n = 0
            for op in self.streams[e]:
                if op.slot is None and op.signal:
                    n += 1
                    op.tick = n
            nsig[e] = n
        with ExitStack() as es:
            esem = {}
            for e in ENGS:
                k = max(1, (nsig[e] + SEM_CAP - 1) // SEM_CAP)
                esem[e] = [es.enter_context(nc.semaphore(f"s_{e}_{i}")) for i in range(k)]
            ssem = {}
            for i, s in enumerate(self.slot_count):
                ssem[s] = es.enter_context(nc.semaphore(f"d_{i}"))
            block = es.enter_context(nc.Block())

            def run(e, eng):
                for op in self.streams[e]:
                    for d in op.waits:
                        if d.slot is not None:
                            eng.wait_ge(ssem[d.slot], 16 * d.count)
                        else:
                            t = d.tick - 1
                            eng.wait_ge(esem[d.eng][t // SEM_CAP], t % SEM_CAP + 1)
                    if e == "pe" and self.pe_log is not None:
                        cnt = [0]

                        class _Px:
                            def matmul(_s, *a, **k):
                                cnt[0] += 1
                                return eng.matmul(*a, **k)

                            def transpose(_s, *a, **k):
                                cnt[0] += 1
                                return eng.transpose(*a, **k)
                        ins = op.fn(_Px())
                        self.pe_log.append((op.tag, cnt[0]))
                    else:
                        ins = op.fn(eng)
                    if op.slot is not None:
                        ins.then_inc(ssem[op.slot], 16)
                    elif op.signal:
                        t = op.tick - 1
                        ins.then_inc(esem[e][t // SEM_CAP], 1)
                if e == "sp":
                    last = {}
                    for d in self.out_dmas:
                        last[d.slot] = max(last.get(d.slot, 0), d.count)
                    for s, c in last.items():
                        eng.wait_ge(ssem[s], 16 * c)

            @block.tensor
            def _(eng):
                run("pe", eng)

            @block.scalar
            def _(eng):
                run("act", eng)

            @block.vector
            def _(eng):
                run("dve", eng)

            @block.gpsimd
            def _(eng):
                run("pool", eng)

            @block.sync
            def _(eng):
                run("sp", eng)
        return nsig


D = 1024
DFF = 2816
NFF = 22
MT = 1024
NBLK = 8
EPS = 1e-6
O_AQ, O_AK, O_AV, O_RQ, O_RK, O_RV, O_RG, O_GA, O_GR = 0, 512, 640, 768, 1280, 1792, 2816, 3840, 4864
NCH = 29
C_ID, C_ONE, C_DEC, C_DK, C_DQ, C32_N = 0, 128, 256, 768, 1280, 1792
B_ONESN, B_ONES256, B_OPAD, B_E, B_ID, CBF_N = 0, 128, 256, 512, 2560, 2688


def make_consts():
    c32 = np.zeros((128, C32_N), np.float32)
    c32[:, C_ID:C_ID + 128] = np.eye(128, dtype=np.float32)
    c32[:, C_ONE:C_ONE + 128] = 1.0
    log_g = np.log(1.0 - np.exp2(-5.0 - np.arange(4, dtype=np.float64)))
    j = np.arange(128)[:, None].astype(np.float64)
    i = np.arange(128)[None, :].astype(np.float64)
    for h in range(4):
        dec = np.where(i >= j, np.exp(log_g[h] * np.maximum(i - j, 0.0)), 0.0) * (128.0 ** -0.5)
        c32[:, C_DEC + h * 128:C_DEC + (h + 1) * 128] = dec
        c32[:, C_DK + h * 128:C_DK + (h + 1) * 128] = (np.exp(log_g[h] * (127.0 - j)) * (128.0 ** -0.5))
        c32[:, C_DQ + h * 128:C_DQ + (h + 1) * 128] = np.exp(log_g[h] * (i + 1.0))
    gC = [float(np.exp(log_g[h] * 128.0)) for h in range(4)]
    cbf = np.zeros((128, CBF_N), np.float32)
    cbf[:, B_ONESN:B_ONESN + 128] = 1.0 / 1024
    cbf[:, B_ONES256:B_ONES256 + 128] = 1.0 / 256
    cbf[:, B_OPAD:B_OPAD + 64] = 1.0
    cbf[:, B_OPAD + 128 + 64:B_OPAD + 256] = 1.0
    cbf[:, B_ID:B_ID + 128] = np.eye(128, dtype=np.float32)
    slopes = np.exp2(-8.0 * (np.arange(8, dtype=np.float64) + 1.0) / 8)
    for g in range(2):
        for half in range(2):
            for p in range(2):
                for cc in range(2):
                    hd = 4 * g + 2 * cc + p
                    dist = (i + 128.0 - j) if half == 0 else (i - j)
                    val = np.where((dist >= 0) & (dist < 128), np.exp(-slopes[hd] * dist), 0.0)
                    o = B_E + g * 1024 + half * 512 + p * 256 + cc * 128
                    cbf[:, o:o + 128] = val
    return c32, cbf, gC


def prm_layout(depth):
    return 28 * depth + 8


def build(nseq, seq, depth):
    assert seq % MT == 0
    mt_per_seq = seq // MT
    nc = bass.Bass("TRN2", target_bir_lowering=False)
    x_d = nc.dram_tensor("x", [nseq, seq, D], F32, kind="ExternalInput").ap()
    w_in = nc.dram_tensor("w_in", [depth, D, 5888], F32, kind="ExternalInput").ap()
    w_ao = nc.dram_tensor("w_att_o", [depth, 512, D], F32, kind="ExternalInput").ap()
    w_ro = nc.dram_tensor("w_ret_o", [depth, D, D], F32, kind="ExternalInput").ap()
    w_o = nc.dram_tensor("w_out", [depth, D, D], F32, kind="ExternalInput").ap()
    w_g = nc.dram_tensor("w_gate", [depth, D, DFF], F32, kind="ExternalInput").ap()
    w_u = nc.dram_tensor("w_up", [depth, D, DFF], F32, kind="ExternalInput").ap()
    w_d = nc.dram_tensor("w_down", [depth, DFF, D], F32, kind="ExternalInput").ap()
    c32_d = nc.dram_tensor("c32", [128, C32_N], F32, kind="ExternalInput").ap()
    cbf_d = nc.dram_tensor("cbf", [128, CBF_N], F32, kind="ExternalInput").ap()
    NP = prm_layout(depth)
    prm_d = nc.dram_tensor("prm", [128, NP], F32, kind="ExternalInput").ap()
    y_d = nc.dram_tensor("y", [nseq, seq, D], F32, kind="ExternalOutput").ap()
    _, _, gC = make_consts()

    P = Prog()
    with ExitStack() as es:
        def sb(name, shape, dt):
            return es.enter_context(nc.sbuf_tensor(name, shape, dt))

        xT = sb("xT", [128, 8, MT], F32)
        hT = sb("hT", [128, 8, MT], BF16)
        A = sb("arena", [128, NCH * 1024], BF16)
        NBUF = 5
        wbuf = [sb(f"wb{i}", [128, 4096], BF16) for i in range(NBUF)]
        c32 = sb("c32s", [128, C32_N], F32)
        cbf = sb("cbfs", [128, CBF_N], BF16)
        prm = sb("prms", [128, NP], F32)
        esink = sb("esink", [128, 4 * depth], F32)
        epsb = sb("epsb", [128, 1], F32)
        vpad = sb("vpad", [128, 9 * 512], BF16)
        vcarry = [sb(f"vcar{l}", [128, 512], BF16) for l in range(depth)]
        akcarry = [sb(f"akcar{l}", [128, 4, 128], BF16) for l in range(depth)]
        state = [sb(f"state{l}", [128, 1024], F32) for l in range(depth)]
        state_bf = sb("state_bf", [128, 1024], BF16)
        stg = [A[:, i * 2048:(i + 1) * 2048].bitcast(F32) for i in range(2)]
        sqt = [sb(f"sqt{i}", [128, 512], BF16) for i in range(2)]
        rinv = sb("rinv", [128, 512], F32)
        etmp2 = sb("etmp2", [128, 1024], F32)
        gtmp = [etmp2[:, i * 512:(i + 1) * 512] for i in range(2)]
        ST2 = [sb(f"ST{i}", [128, 512], BF16) for i in range(2)]
        qd2 = [sb(f"qdT{i}", [128, 512], BF16) for i in range(2)]
        etmp = sb("etmp", [128, 1024], F32)
        pT = [sb(f"pT{i}", [128, 1024], BF16) for i in range(2)]
        den = sb("den", [128, 512], F32)
        osq = [sb(f"osq{i}", [128, 512], BF16) for i in range(2)]
        pt = [es.enter_context(nc.psum_tensor(f"pt{i}", [128, 1024], F32)) for i in range(4)]

        def bank(i):
            return pt[i // 2][:, (i % 2) * 512:(i % 2) * 512 + 512]

        def bk(i):
            return ("ps", i)

        bank_ctr = [0]

        def nextbank():
            b = bank_ctr[0] % 8
            bank_ctr[0] += 1
            return b

        def AR(ch, lo=0, hi=1024):
            return A[:, ch * 1024 + lo:ch * 1024 + hi]

        def akeys(lo, hi):
            return [("ar", g) for g in range(lo // 512, (hi - 1) // 512 + 1)]

        def ark(ch, lo=0, hi=1024):
            return akeys(ch * 1024 + lo, ch * 1024 + hi)

        def AR3(ch0, n, lo, hi):
            return A[:, ch0 * 1024:(ch0 + n) * 1024].rearrange("p (c t) -> p c t", c=n)[:, :, lo:hi]

        def ark3(ch0, n, lo, hi):
            ks = []
            for c in range(n):
                ks += ark(ch0 + c, lo, hi)
            return ks

        KPB = 8 * 1024

        def KP(q, lo, hi):
            return A[:, KPB + q * 1152 + lo:KPB + q * 1152 + hi]

        def kpk(q, lo, hi):
            return akeys(KPB + q * 1152 + lo, KPB + q * 1152 + hi)

        def tts(tt):
            return slice(tt * 512, tt * 512 + 512)

        eng_rr = [0]

        def evac_eng():
            eng_rr[0] += 1
            return "act" if eng_rr[0] % 2 else "dve"

        def copy_op(eng, out, in_, reads, writes, scale=None):
            if eng == "act":
                if scale is None:
                    P.add("act", lambda e: e.activation(out=out, in_=in_, func=AF.Copy), reads=reads, writes=writes)
                else:
                    P.add("act", lambda e: e.activation(out=out, in_=in_, func=AF.Copy, scale=scale),
                          reads=reads, writes=writes)
            else:
                assert scale is None
                P.add("dve", lambda e: e.tensor_copy(out=out, in_=in_), reads=reads, writes=writes)

        def mm_group(out_ap, pairs, reads, bkeys):
            def f(e):
                n = len(pairs)
                ins = None
                for i, (l, r) in enumerate(pairs):
                    ins = e.matmul(out_ap, l, r, start=(i == 0), stop=(i == n - 1))
                return ins
            P.add("pe", f, reads=reads, writes=bkeys)

        def mm_multi(groups, reads, bkeys):
            def f(e):
                ins = None
                for out_ap, pairs in groups:
                    n = len(pairs)
                    for i, (l, r) in enumerate(pairs):
                        ins = e.matmul(out_ap, l, r, start=(i == 0), stop=(i == n - 1))
                return ins
            P.add("pe", f, reads=reads, writes=bkeys)

        hkeys = {tt: [("hT", c, tt) for c in range(8)] for tt in range(2)}

        P.add("sp", lambda e: e.dma_start(out=c32[:, :], in_=c32_d[:, :]), writes=["c32"], slot="c32")
        P.add("sp", lambda e: e.dma_start(out=prm[:, :], in_=prm_d[:, :]), writes=["prm"], slot="prm")
        P.add("pool", lambda e: e.dma_start(out=cbf[:, :], in_=cbf_d[:, :]), writes=["cbf"], slot="cbf")
        P.add("dve", lambda e: e.memset(epsb[:, :], EPS), writes=["epsb"])
        P.add("dve", lambda e: e.memset(vpad[:, :], 0.0), writes=["vpad"])
        for l in range(depth):
            P.add("act", lambda e, l=l: e.activation(out=esink[:, 4 * l:4 * l + 4],
                                                     in_=prm[:, 28 * l + 24:28 * l + 28], func=AF.Exp),
                  reads=["prm"], writes=[("esink", l)])

        units = []

        def unit_simple(src, KC, ncols):
            return [(0, KC, ncols, 0, ncols, src.rearrange("(k p) n -> p k n", p=128))]

        items = []

        loaded = [0]
        all_units = []

        def emit_load(ui):
            pieces = all_units[ui]
            j = ui % NBUF
            for pi, (dlo, KC, ncols, clo, cn, src) in enumerate(pieces):
                dst = wbuf[j][:, dlo:dlo + KC * ncols].rearrange("p (k n) -> p k n", k=KC)[:, :, clo:clo + cn]
                P.add("pool", lambda e, dst=dst, src=src: e.dma_start(out=dst, in_=src),
                      writes=[("w", j)], slot=("w", j, pi))

        def run_items():
            base = 0
            for us, _ in items:
                all_units.extend(us)
            ui = 0
            for us, fn in items:
                need = min(len(all_units), ui + NBUF)
                while loaded[0] < need:
                    emit_load(loaded[0])
                    loaded[0] += 1
                views = []
                for k, u in enumerate(us):
                    j = (ui + k) % NBUF
                    _, KC, ncols, _, _, _ = u[0]
                    views.append((wbuf[j][:, 0:KC * ncols].rearrange("p (k n) -> p k n", k=KC), ("w", j)))
                P.tag = getattr(fn, "__name__", "?")
                fn(views)
                ui += len(us)

        def norm_stage(gcol, final=False):
            def f_norm(_):
                for tt in range(2):
                    b = nextbank()
                    for c in range(8):
                        s = sqt[c % 2]
                        P.add("act", lambda e, s=s, c=c, tt=tt: e.activation(out=s[:, :], in_=xT[:, c, tts(tt)],
                                                                             func=AF.Square),
                              reads=[("xT", c, tt)], writes=[("sqt", c % 2)])
                        P.add("pe", lambda e, s=s, c=c, b=b: e.matmul(bank(b), cbf[:, B_ONESN:B_ONESN + 128], s[:, :],
                                                                      start=(c == 0), stop=(c == 7)),
                              reads=[("sqt", c % 2), "cbf"], writes=[bk(b)])
                    rb, rbk = (rinv, "rinv") if tt == 0 else (den, "den")
                    P.add("act", lambda e, b=b, rb=rb: e.activation(out=rb[:, :], in_=bank(b), func=AF.Ln,
                                                                    bias=epsb[:, 0:1]),
                          reads=[bk(b), "epsb"], writes=[rbk])
                    P.add("act", lambda e, rb=rb: e.activation(out=rb[:, :], in_=rb[:, :], func=AF.Exp, scale=-0.5),
                          reads=[rbk], writes=[rbk])
                    for c in range(8):
                        if final:
                            P.add("dve", lambda e, c=c, tt=tt, rb=rb: e.scalar_tensor_tensor(
                                out=xT[:, c, tts(tt)], in0=xT[:, c, tts(tt)], scalar=prm[:, gcol + c:gcol + c + 1],
                                in1=rb[:, :], op0=ALU.mult, op1=ALU.mult),
                                reads=[("xT", c, tt), "prm", rbk], writes=[("xT", c, tt)])
                        else:
                            P.add("dve", lambda e, c=c, tt=tt, rb=rb: e.scalar_tensor_tensor(
                                out=hT[:, c, tts(tt)], in0=xT[:, c, tts(tt)], scalar=prm[:, gcol + c:gcol + c + 1],
                                in1=rb[:, :], op0=ALU.mult, op1=ALU.mult),
                                reads=[("xT", c, tt), "prm", rbk], writes=[("hT", c, tt)])
            items.append(([], f_norm))

        def fm_proj(wv, wk, ci, tt, KC, src, srckeys, b=None):
            if b is None:
                b = nextbank()
            mm_group(bank(b), [(wv[:, k, ci * 128:(ci + 1) * 128], src(k, tt)) for k in range(KC)],
                     reads=[wk] + srckeys, bkeys=[bk(b)])
            return b

        def h_src(k, tt):
            return hT[:, k, tts(tt)]

        def mixer_stage(l, first_mt, last_mt):
            wl = w_in[l]
            pb = 28 * l

            def f_rq(views):
                (wv, wk), = views
                for tt in range(2):
                    for h in range(4):
                        b = fm_proj(wv, wk, h, tt, 8, h_src, hkeys[tt])
                        copy_op(evac_eng(), AR(h, tt * 512, tt * 512 + 512), bank(b), [bk(b)], ark(h, tt * 512, tt * 512 + 512))
            items.append(([unit_simple(wl[:, O_RQ:O_RQ + 512], 8, 512)], f_rq))

            def f_rk(views):
                (wv, wk), = views
                for h in range(4):
                    for tt in range(2):
                        b = fm_proj(wv, wk, h, tt, 8, h_src, hkeys[tt])
                        copy_op(evac_eng(), AR(4 + h, tt * 512, tt * 512 + 512), bank(b), [bk(b)],
                                ark(4 + h, tt * 512, tt * 512 + 512))
            items.append(([unit_simple(wl[:, O_RK:O_RK + 512], 8, 512)], f_rk))

            def rk_tm_transposes():
                for blk in range(NBLK):
                    b = nextbank()
                    mm_multi([(bank(b)[:, h * 128:(h + 1) * 128],
                               [(AR(4 + h, blk * 128, blk * 128 + 128), cbf[:, B_ID:B_ID + 128])]) for h in range(4)],
                             reads=ark3(4, 4, blk * 128, blk * 128 + 128) + ["cbf"], bkeys=[bk(b)])
                    lo = (blk % 2) * 512
                    P.add("dve", lambda e, b=b, blk=blk, lo=lo: e.tensor_tensor(
                        out=AR(8 + blk // 2, lo, lo + 512), in0=bank(b), in1=c32[:, C_DK:C_DK + 512], op=ALU.mult),
                        reads=[bk(b), "c32"], writes=ark(8 + blk // 2, lo, lo + 512))

            for half in range(2):
                def f_rv(views, half=half):
                    (wv, wk), = views
                    for blk in range(NBLK):
                        b = nextbank()
                        tt = blk // 4
                        mm_group(bank(b), [(hT[:, k, blk * 128:(blk + 1) * 128], wv[:, k, :]) for k in range(8)],
                                 reads=[wk] + hkeys[tt], bkeys=[bk(b)])
                        copy_op(evac_eng(), AR(12 + blk, half * 512, half * 512 + 512), bank(b), [bk(b)],
                                ark(12 + blk, half * 512, half * 512 + 512))
                    if half == 1:
                        rk_tm_transposes()
                items.append(([unit_simple(wl[:, O_RV + half * 512:O_RV + half * 512 + 512], 8, 512)], f_rv))

            def f_ret(_):
                if not first_mt:
                    P.add("act", lambda e: e.activation(out=state_bf[:, :], in_=state[l][:, :], func=AF.Copy),
                          reads=[("state", l)], writes=["state_bf"])

                def isfirst(blk):
                    return first_mt and blk == 0

                def islast(blk):
                    return last_mt and blk == NBLK - 1

                def pe_front(blk):
                    sb_ = blk % 2
                    mm_multi([(bank(sb_)[:, h * 128:(h + 1) * 128], [(AR(4 + h, blk * 128, blk * 128 + 128),
                                                                      AR(h, blk * 128, blk * 128 + 128))]) for h in range(4)],
                             reads=ark3(0, 8, blk * 128, blk * 128 + 128), bkeys=[bk(sb_)])
                    if not islast(blk):
                        lo = (blk % 2) * 512
                        su = pt[2 + blk % 2]
                        mm_multi([(su[:, h * 256:(h + 1) * 256],
                                   [(AR(8 + blk // 2, lo + h * 128, lo + h * 128 + 128), AR(12 + blk, h * 256, h * 256 + 256))])
                                  for h in range(4)],
                                 reads=ark(8 + blk // 2, lo, lo + 512) + ark(12 + blk),
                                 bkeys=[bk(4 + 2 * (blk % 2)), bk(5 + 2 * (blk % 2))])

                def dve_front(blk):
                    sb_ = blk % 2
                    STb = ST2[blk % 2]
                    P.add("dve", lambda e: e.tensor_tensor(out=STb[:, :], in0=bank(sb_), in1=c32[:, C_DEC:C_DEC + 512],
                                                           op=ALU.mult),
                          reads=[bk(sb_), "c32"], writes=[("ST", blk % 2)])
                    if not isfirst(blk):
                        qb = qd2[blk % 2]
                        P.add("dve", lambda e: e.tensor_tensor(
                            out=qb[:, :].rearrange("p (h i) -> p h i", h=4), in0=AR3(0, 4, blk * 128, blk * 128 + 128),
                            in1=c32[:, C_DQ:C_DQ + 512].rearrange("p (h i) -> p h i", h=4), op=ALU.mult),
                            reads=ark3(0, 4, blk * 128, blk * 128 + 128) + ["c32"], writes=[("qdT", blk % 2)])

                def back(blk):
                    first = isfirst(blk)
                    STb = ST2[blk % 2]
                    qb = qd2[blk % 2]
                    groups = []
                    for h in range(4):
                        for jj in range(2):
                            c = 2 * h + jj
                            pairs = [(AR(12 + blk, h * 256 + jj * 128, h * 256 + jj * 128 + 128), STb[:, h * 128:(h + 1) * 128])]
                            if not first:
                                pairs.append((state_bf[:, h * 256 + jj * 128:h * 256 + jj * 128 + 128],
                                              qb[:, h * 128:(h + 1) * 128]))
                            groups.append((pt[1][:, c * 128:(c + 1) * 128], pairs))
                    mm_multi(groups, reads=ark(12 + blk) + [("ST", blk % 2)] + ([] if first else ["state_bf", ("qdT", blk % 2)]),
                             bkeys=[bk(2), bk(3)])
                    copy_op("act", AR3(21, 8, blk * 128, blk * 128 + 128), pt[1][:, :].rearrange("p (c t) -> p c t", c=8),
                            [bk(2), bk(3)], ark3(21, 8, blk * 128, blk * 128 + 128))
                    if not islast(blk):
                        su = pt[2 + blk % 2]
                        sk = [bk(4 + 2 * (blk % 2)), bk(5 + 2 * (blk % 2))]
                        if first:
                            P.add("dve", lambda e: e.tensor_copy(out=state[l][:, :], in_=su[:, :]),
                                  reads=sk, writes=[("state", l)])
                        else:
                            for h in range(4):
                                P.add("dve", lambda e, h=h: e.scalar_tensor_tensor(
                                    out=state[l][:, h * 256:(h + 1) * 256], in0=state[l][:, h * 256:(h + 1) * 256],
                                    scalar=gC[h], in1=su[:, h * 256:(h + 1) * 256], op0=ALU.mult, op1=ALU.add),
                                    reads=sk + [("state", l)], writes=[("state", l)])
                        P.add("act", lambda e: e.activation(out=state_bf[:, :], in_=state[l][:, :], func=AF.Copy),
                              reads=[("state", l)], writes=["state_bf"])

                pe_front(0)
                for blk in range(NBLK):
                    dve_front(blk)
                    if blk + 1 < NBLK:
                        pe_front(blk + 1)
                    back(blk)
            items.append(([], f_ret))

            akv_src = lambda c0, n: wl[:, c0:c0 + n].rearrange("(k p) n -> p k n", p=128)
            akv_unit = [(0, 8, 384, 0, 64, akv_src(O_AK, 64)), (0, 8, 384, 64, 64, akv_src(O_AK, 64)),
                        (0, 8, 384, 128, 64, akv_src(O_AK + 64, 64)), (0, 8, 384, 192, 64, akv_src(O_AK + 64, 64)),
                        (0, 8, 384, 256, 128, akv_src(O_AV, 128))]

            def f_gn(views):
                (wq, wqk), (wkv, wkvk) = views
                fillers = []
                for c in range(4):
                    for tt in range(2):
                        def f_aq_tile(c=c, tt=tt):
                            lo, hi = tt * 512, tt * 512 + 512
                            b = fm_proj(wq, wqk, c, tt, 8, h_src, hkeys[tt])
                            return lambda: copy_op(evac_eng(), AR(c, lo, hi), bank(b), [bk(b)], ark(c, lo, hi))
                        fillers.append(f_aq_tile)

                def akv_prep():
                    for q in range(4):
                        p = q % 2
                        rows = slice(64, 128) if p == 0 else slice(0, 64)
                        P.add("dve", lambda e, q=q, rows=rows: e.memset(A[rows, KPB + q * 1152:KPB + (q + 1) * 1152], 0.0),
                              writes=kpk(q, 0, 1152))
                    if not first_mt:
                        P.add("dve", lambda e: e.tensor_copy(
                            out=A[:, KPB:KPB + 4 * 1152].rearrange("p (q t) -> p q t", q=4)[:, :, 0:128], in_=akcarry[l][:, :, :]),
                            reads=[("akcar", l)], writes=[k for q in range(4) for k in kpk(q, 0, 128)])
                        P.add("dve", lambda e: e.tensor_copy(out=vpad[:, 0:512], in_=vcarry[l][:, :]),
                              reads=[("vcar", l)], writes=["vpad"])

                for g in range(2):
                    for tt in range(2):
                        def f_k_tile(g=g, tt=tt):
                            b = fm_proj(wkv, wkvk, g, tt, 8, h_src, hkeys[tt])
                            lo = 128 + tt * 512

                            def ev():
                                P.add("act", lambda e: e.activation(out=A[0:64, KPB + (2 * g) * 1152 + lo:KPB + (2 * g) * 1152 + lo + 512],
                                                                    in_=bank(b)[0:64, :], func=AF.Copy),
                                      reads=[bk(b)], writes=kpk(2 * g, lo, lo + 512))
                                P.add("dve", lambda e: e.tensor_copy(
                                    out=A[64:128, KPB + (2 * g + 1) * 1152 + lo:KPB + (2 * g + 1) * 1152 + lo + 512], in_=bank(b)[64:128, :]),
                                    reads=[bk(b)], writes=kpk(2 * g + 1, lo, lo + 512))
                            return ev
                        fillers.append(f_k_tile)
                for bq in range(2):
                    def f_v_tile(bq=bq):
                        b = nextbank()
                        groups = []
                        for bi in range(4):
                            blk = bq * 4 + bi
                            groups.append((bank(b)[:, bi * 128:(bi + 1) * 128],
                                           [(hT[:, k, blk * 128:(blk + 1) * 128], wkv[:, k, 256:384]) for k in range(8)]))
                        mm_multi(groups, reads=[wkvk] + hkeys[bq], bkeys=[bk(b)])

                        def ev():
                            vv = vpad[:, :].rearrange("p (s q d) -> p s q d", s=9, q=4)
                            src4 = bank(b).rearrange("p (s d) -> p s d", s=4)
                            for g in range(2):
                                for p in range(2):
                                    dst = vv[:, 1 + bq * 4:1 + bq * 4 + 4, 2 * g + p, 64 * p:64 * p + 64]
                                    srcv = src4[:, :, g * 64:(g + 1) * 64]
                                    copy_op(evac_eng(), dst, srcv, [bk(b)], ["vpad"])
                        return ev
                    fillers.append(f_v_tile)

                pending = []

                def fill(n):
                    for _ in range(n):
                        if fillers:
                            pending.append(fillers.pop(0)())

                def drain():
                    while pending:
                        pending.pop(0)()

                akv_prep()
                slot = 0
                for h in range(4):
                    for tt in range(2):
                        lo, hi = tt * 512, tt * 512 + 512
                        bm, bq = nextbank(), nextbank()
                        for jj in range(2):
                            c = 21 + 2 * h + jj
                            P.add("act", lambda e, c=c, jj=jj, lo=lo, hi=hi: e.activation(out=osq[jj][:, :], in_=AR(c, lo, hi),
                                                                                          func=AF.Square),
                                  reads=ark(c, lo, hi), writes=[("osq", jj)])
                        mm_group(bank(bm), [(cbf[:, B_ONES256:B_ONES256 + 128], AR(21 + 2 * h + jj, lo, hi)) for jj in range(2)],
                                 reads=["cbf"] + ark(21 + 2 * h, lo, hi) + ark(22 + 2 * h, lo, hi), bkeys=[bk(bm)])
                        mm_group(bank(bq), [(cbf[:, B_ONES256:B_ONES256 + 128], osq[jj][:, :]) for jj in range(2)],
                                 reads=["cbf", ("osq", 0), ("osq", 1)], bkeys=[bk(bq)])
                        fill(2 if slot % 4 != 3 else 1)
                        slot += 1
                        P.add("act", lambda e, bm=bm: e.activation(out=etmp[:, 0:512], in_=bank(bm), func=AF.Square),
                              reads=[bk(bm)], writes=[("etmp", 0)])
                        P.add("dve", lambda e, bq=bq: e.tensor_tensor(out=etmp[:, 512:1024], in0=bank(bq), in1=etmp[:, 0:512],
                                                                      op=ALU.subtract),
                              reads=[bk(bq), ("etmp", 0)], writes=[("etmp", 1)])
                        P.add("act", lambda e: e.activation(out=etmp[:, 512:1024], in_=etmp[:, 512:1024], func=AF.Ln,
                                                            bias=epsb[:, 0:1]),
                              reads=[("etmp", 1), "epsb"], writes=[("etmp", 1)])
                        P.add("act", lambda e: e.activation(out=etmp[:, 512:1024], in_=etmp[:, 512:1024], func=AF.Exp,
                                                            scale=-0.5),
                              reads=[("etmp", 1)], writes=[("etmp", 1)])
                        for jj in range(2):
                            c = 21 + 2 * h + jj
                            gcol = pb + 16 + 2 * h + jj
                            gt = gtmp[jj]
                            P.add("dve", lambda e, c=c, gt=gt, bm=bm, lo=lo, hi=hi: e.tensor_tensor(
                                out=gt[:, :], in0=AR(c, lo, hi), in1=bank(bm), op=ALU.subtract),
                                reads=ark(c, lo, hi) + [bk(bm)], writes=[("gtmp", jj)])
                            P.add("dve", lambda e, c=c, gt=gt, gcol=gcol, lo=lo, hi=hi: e.scalar_tensor_tensor(
                                out=AR(c, lo, hi), in0=gt[:, :], scalar=prm[:, gcol:gcol + 1], in1=etmp[:, 512:1024],
                                op0=ALU.mult, op1=ALU.mult),
                                reads=[("gtmp", jj), "prm", ("etmp", 1)], writes=ark(c, lo, hi))
                        drain()
                while fillers:
                    fill(1)
                    drain()
            items.append(([unit_simple(wl[:, O_AQ:O_AQ + 512], 8, 512), akv_unit], f_gn))

            for u in range(2):
                def f_rg(views, u=u):
                    (wv, wk), = views
                    for ci in range(4):
                        c = u * 4 + ci
                        for tt in range(2):
                            lo, hi = tt * 512, tt * 512 + 512
                            b = fm_proj(wv, wk, ci, tt, 8, h_src, hkeys[tt])
                            gi = (c * 2 + tt) % 2
                            gt = gtmp[gi]
                            P.add("act", lambda e, b=b, gt=gt: e.activation(out=gt[:, :], in_=bank(b), func=AF.Silu),
                                  reads=[bk(b)], writes=[("gtmp", gi)])
                            P.add("dve", lambda e, c=c, gt=gt, lo=lo, hi=hi: e.tensor_tensor(
                                out=AR(21 + c, lo, hi), in0=AR(21 + c, lo, hi), in1=gt[:, :], op=ALU.mult),
                                reads=ark(21 + c, lo, hi) + [("gtmp", gi)], writes=ark(21 + c, lo, hi))
                items.append(([unit_simple(wl[:, O_RG + u * 512:O_RG + u * 512 + 512], 8, 512)], f_rg))

            def f_att(_):
                vv = vpad[:, :].rearrange("p (s q d) -> p s q d", s=9, q=4)

                def halves_of(blk):
                    return [1] if (first_mt and blk == 0) else [0, 1]

                def S(blk, g):
                    groups = []
                    rk_ = []
                    for half in halves_of(blk):
                        for p in range(2):
                            q = 2 * g + p
                            kc = (blk + half) * 128
                            o = half * 512 + p * 256
                            groups.append((pt[g][:, o:o + 256],
                                           [(KP(q, kc, kc + 128), AR3(2 * g, 2, blk * 128, blk * 128 + 128))]))
                            rk_ += kpk(q, kc, kc + 128)
                    mm_multi(groups, reads=rk_ + ark3(0, 4, blk * 128, blk * 128 + 128), bkeys=[bk(2 * g), bk(2 * g + 1)])

                def softmax(blk, g):
                    lo = 512 if (first_mt and blk == 0) else 0
                    eb = etmp if g == 0 else etmp2
                    ek = [("etmp", 0), ("etmp", 1)] if g == 0 else [("gtmp", 0), ("gtmp", 1)]
                    P.add("act", lambda e: e.activation(out=eb[:, lo:1024], in_=pt[g][:, lo:1024], func=AF.Exp, scale=0.125),
                          reads=[bk(2 * g), bk(2 * g + 1)], writes=ek)
                    pg = pT[g]
                    P.add("dve", lambda e: e.tensor_tensor(
                        out=pg[:, lo:1024], in0=eb[:, lo:1024], in1=cbf[:, B_E + g * 1024 + lo:B_E + g * 1024 + 1024],
                        op=ALU.mult),
                        reads=ek + ["cbf"], writes=[("pT", g)])

                def PV(blk, g):
                    pg = pT[g]
                    ob = pt[2 + blk % 2]
                    pv, dn = [], []
                    for half in halves_of(blk):
                        for p in range(2):
                            o = half * 512 + p * 256
                            pv.append((vv[:, blk + half, 2 * g + p, :], pg[:, o:o + 256]))
                            dn.append((cbf[:, B_OPAD + p * 128:B_OPAD + p * 128 + 128], pg[:, o:o + 256]))
                    mm_multi([(ob[:, g * 256:g * 256 + 256], pv), (ob[:, 512 + g * 256:512 + g * 256 + 256], dn)],
                             reads=["vpad", "cbf", ("pT", g)], bkeys=[bk(4 + 2 * (blk % 2)), bk(5 + 2 * (blk % 2))])

                def fin(blk):
                    ob = pt[2 + blk % 2]
                    kb = [bk(4 + 2 * (blk % 2)), bk(5 + 2 * (blk % 2))]
                    for c in range(4):
                        P.add("act", lambda e, c=c: e.activation(
                            out=den[:, c * 128:(c + 1) * 128], in_=ob[:, 512 + c * 128:512 + (c + 1) * 128], func=AF.Ln,
                            bias=esink[:, 4 * l + c:4 * l + c + 1]),
                            reads=kb + [("esink", l)], writes=["den"])
                    P.add("act", lambda e: e.activation(out=den[:, :], in_=den[:, :], func=AF.Exp, scale=-1.0),
                          reads=["den"], writes=["den"])
                    P.add("dve", lambda e: e.tensor_tensor(
                        out=AR3(4, 4, blk * 128, blk * 128 + 128), in0=ob[:, 0:512].rearrange("p (c t) -> p c t", c=4),
                        in1=den[:, :].rearrange("p (c t) -> p c t", c=4), op=ALU.mult),
                        reads=kb + ["den"], writes=ark3(4, 4, blk * 128, blk * 128 + 128))

                S(0, 0)
                S(0, 1)
                softmax(0, 0)
                softmax(0, 1)
                for blk in range(NBLK):
                    PV(blk, 0)
                    if blk + 1 < NBLK:
                        S(blk + 1, 0)
                    PV(blk, 1)
                    if blk + 1 < NBLK:
                        S(blk + 1, 1)
                        softmax(blk + 1, 0)
                        softmax(blk + 1, 1)
                    fin(blk)
                if not last_mt:
                    P.add("dve", lambda e: e.tensor_copy(
                        out=akcarry[l][:, :, :], in_=A[:, KPB:KPB + 4 * 1152].rearrange("p (q t) -> p q t", q=4)[:, :, 1024:1152]),
                        reads=[k for q in range(4) for k in kpk(q, 1024, 1152)], writes=[("akcar", l)])
                    P.add("dve", lambda e: e.tensor_copy(out=vcarry[l][:, :], in_=vpad[:, 8 * 512:9 * 512]),
                          reads=["vpad"], writes=[("vcar", l)])
            items.append(([], f_att))

            for u in range(2):
                def f_ao(views, u=u):
                    (wa, wak), (wg, wgk) = views
                    for ci in range(4):
                        c = u * 4 + ci
                        for tt in range(2):
                            lo, hi = tt * 512, tt * 512 + 512
                            ba = fm_proj(wa, wak, ci, tt, 4, lambda k, tt: AR(4 + k, tt * 512, tt * 512 + 512),
                                         ark3(4, 4, lo, hi))
                            bg = fm_proj(wg, wgk, ci, tt, 8, h_src, hkeys[tt])
                            gi = (c * 2 + tt) % 2
                            gt = gtmp[gi]
                            P.add("act", lambda e, bg=bg, gt=gt: e.activation(out=gt[:, :], in_=bank(bg), func=AF.Sigmoid),
                                  reads=[bk(bg)], writes=[("gtmp", gi)])
                            P.add("dve", lambda e, c=c, ba=ba, gt=gt, lo=lo, hi=hi: e.tensor_tensor(
                                out=AR(13 + c, lo, hi), in0=bank(ba), in1=gt[:, :], op=ALU.mult),
                                reads=[bk(ba), ("gtmp", gi)], writes=ark(13 + c, lo, hi))
                items.append(([unit_simple(w_ao[l][:, u * 512:u * 512 + 512], 4, 512),
                               unit_simple(wl[:, O_GA + u * 512:O_GA + u * 512 + 512], 8, 512)], f_ao))

            for u in range(2):
                def f_ro(views, u=u):
                    (wr, wrk), (wg, wgk) = views
                    for ci in range(4):
                        c = u * 4 + ci
                        for tt in range(2):
                            lo, hi = tt * 512, tt * 512 + 512
                            br = fm_proj(wr, wrk, ci, tt, 8, lambda k, tt: AR(21 + k, tt * 512, tt * 512 + 512),
                                         ark3(21, 8, lo, hi))
                            bg = fm_proj(wg, wgk, ci, tt, 8, h_src, hkeys[tt])
                            gi = (c * 2 + tt) % 2
                            gt = gtmp[gi]
                            P.add("act", lambda e, bg=bg, gt=gt: e.activation(out=gt[:, :], in_=bank(bg), func=AF.Sigmoid),
                                  reads=[bk(bg)], writes=[("gtmp", gi)])
                            P.add("dve", lambda e, br=br, gt=gt: e.tensor_tensor(
                                out=gt[:, :], in0=bank(br), in1=gt[:, :], op=ALU.mult),
                                reads=[bk(br), ("gtmp", gi)], writes=[("gtmp", gi)])
                            P.add("dve", lambda e, c=c, gt=gt, lo=lo, hi=hi: e.tensor_tensor(
                                out=AR(13 + c, lo, hi), in0=AR(13 + c, lo, hi), in1=gt[:, :], op=ALU.add),
                                reads=ark(13 + c, lo, hi) + [("gtmp", gi)], writes=ark(13 + c, lo, hi))
                items.append(([unit_simple(w_ro[l][:, u * 512:u * 512 + 512], 8, 512),
                               unit_simple(wl[:, O_GR + u * 512:O_GR + u * 512 + 512], 8, 512)], f_ro))

            for u in range(2):
                def f_o(views, u=u):
                    (wv, wk), = views
                    for ci in range(4):
                        c = u * 4 + ci
                        for tt in range(2):
                            lo, hi = tt * 512, tt * 512 + 512
                            b = fm_proj(wv, wk, ci, tt, 8, lambda k, tt: AR(13 + k, tt * 512, tt * 512 + 512),
                                        ark3(13, 8, lo, hi))
                            P.add("dve", lambda e, c=c, b=b, tt=tt: e.tensor_tensor(
                                out=xT[:, c, tts(tt)], in0=xT[:, c, tts(tt)], in1=bank(b), op=ALU.add),
                                reads=[bk(b), ("xT", c, tt)], writes=[("xT", c, tt)])
                items.append(([unit_simple(w_o[l][:, u * 512:u * 512 + 512], 8, 512)], f_o))

        def ffn_stage(l):
            norm_stage(28 * l + 8)
            for u in range(6):
                n = 512 if u < 5 else 256

                def f_gu(views, u=u, n=n):
                    (wg, wgk), (wu, wuk) = views
                    for tt in range(2):
                        for ci in range(n // 128):
                            c = u * 4 + ci
                            lo, hi = tt * 512, tt * 512 + 512
                            bg = fm_proj(wg, wgk, ci, tt, 8, h_src, hkeys[tt])
                            bu = fm_proj(wu, wuk, ci, tt, 8, h_src, hkeys[tt])
                            gi = (ci + tt) % 2
                            gt = gtmp[gi]
                            P.add("act", lambda e, bg=bg, gt=gt: e.activation(out=gt[:, :], in_=bank(bg), func=AF.Silu),
                                  reads=[bk(bg)], writes=[("gtmp", gi)])
                            P.add("dve", lambda e, c=c, bu=bu, gt=gt, lo=lo, hi=hi: e.tensor_tensor(
                                out=AR(c, lo, hi), in0=bank(bu), in1=gt[:, :], op=ALU.mult),
                                reads=[bk(bu), ("gtmp", gi)], writes=ark(c, lo, hi))
                items.append(([unit_simple(w_g[l][:, u * 512:u * 512 + n], 8, n),
                               unit_simple(w_u[l][:, u * 512:u * 512 + n], 8, n)], f_gu))
            for c in range(8):
                def f_dn(views, c=c):
                    (wv, wk), = views
                    for tt in range(2):
                        lo, hi = tt * 512, tt * 512 + 512
                        b = nextbank()
                        mm_group(bank(b), [(wv[:, k, :], AR(k, lo, hi)) for k in range(NFF)],
                                 reads=[wk] + ark3(0, NFF, lo, hi), bkeys=[bk(b)])
                        P.add("dve", lambda e, c=c, b=b, tt=tt: e.tensor_tensor(
                            out=xT[:, c, tts(tt)], in0=xT[:, c, tts(tt)], in1=bank(b), op=ALU.add),
                            reads=[bk(b), ("xT", c, tt)], writes=[("xT", c, tt)])
                items.append(([unit_simple(w_d[l][:, c * 128:(c + 1) * 128], NFF, 128)], f_dn))

        stgL = [A[:, (22 + 2 * i) * 1024:(24 + 2 * i) * 1024].bitcast(F32) for i in range(2)]

        def ld_dma(s, t0, blk):
            sg = stgL[blk % 2]
            P.add("sp", lambda e: e.dma_start(out=sg[:, :], in_=x_d[s, t0 + blk * 128:t0 + blk * 128 + 128, :]),
                  writes=ark3(22 + 2 * (blk % 2), 2, 0, 1024), slot=("ld", blk % 2))

        def ld_xpose(blk):
            sg = stgL[blk % 2]
            tt = blk // 4
            for hf in range(2):
                b = nextbank()

                def f(e, hf=hf, b=b):
                    ins = None
                    for jx in range(4):
                        c = hf * 4 + jx
                        ins = e.transpose(bank(b)[:, jx * 128:(jx + 1) * 128], sg[:, c * 128:(c + 1) * 128],
                                          c32[:, C_ID:C_ID + 128])
                    return ins
                P.add("pe", f, reads=ark3(22 + 2 * (blk % 2), 2, 0, 1024) + ["c32"], writes=[bk(b)])
                copy_op(evac_eng(), xT[:, hf * 4:hf * 4 + 4, blk * 128:blk * 128 + 128],
                        bank(b).rearrange("p (c t) -> p c t", c=4), [bk(b)],
                        [("xT", c, tt) for c in range(hf * 4, hf * 4 + 4)])

        def st_block(s, t0, blk):
            sg = stg[blk % 2]
            tt = blk // 4
            for hf in range(2):
                b = nextbank()

                def f(e, hf=hf, b=b):
                    ins = None
                    for jx in range(4):
                        c = hf * 4 + jx
                        ins = e.transpose(bank(b)[:, jx * 128:(jx + 1) * 128], xT[:, c, blk * 128:blk * 128 + 128],
                                          c32[:, C_ID:C_ID + 128])
                    return ins
                P.add("pe", f, reads=[("xT", c, tt) for c in range(hf * 4, hf * 4 + 4)] + ["c32"], writes=[bk(b)])
                copy_op(evac_eng(), sg[:, hf * 512:hf * 512 + 512], bank(b), [bk(b)], ark(2 * (blk % 2) + hf))
            P.add("sp", lambda e: e.dma_start(out=y_d[s, t0 + blk * 128:t0 + blk * 128 + 128, :], in_=sg[:, :]),
                  reads=ark3(2 * (blk % 2), 2, 0, 1024), slot=("st", blk % 2), is_out=True)

        def load_stage(s, t0):
            def f_load(_):
                ld_dma(s, t0, 0)
                ld_dma(s, t0, 1)
                for blk in range(NBLK):
                    ld_xpose(blk)
                    if blk + 2 < NBLK:
                        ld_dma(s, t0, blk + 2)
            items.append(([], f_load))

        def store_load_stage(s, t0, gcol, nxt):
            if nxt is not None:
                def f_pre(_):
                    ld_dma(nxt[0], nxt[1], 0)
                    ld_dma(nxt[0], nxt[1], 1)
                items.append(([], f_pre))
            norm_stage(gcol, final=True)

            def f_store(_):
                for blk in range(4):
                    st_block(s, t0, blk)
                for blk in range(4):
                    if nxt is not None:
                        ld_xpose(blk)
                        ld_dma(nxt[0], nxt[1], blk + 2)
                    st_block(s, t0, 4 + blk)
                if nxt is not None:
                    for blk in range(4, NBLK):
                        ld_xpose(blk)
                        if blk + 2 < NBLK:
                            ld_dma(nxt[0], nxt[1], blk + 2)
            items.append(([], f_store))

        order = [(s, m * MT, m) for s in range(nseq) for m in range(mt_per_seq)]
        load_stage(order[0][0], order[0][1])
        for i, (s, t0, m) in enumerate(order):
            for l in range(depth):
                norm_stage(28 * l)
                mixer_stage(l, m == 0, m == mt_per_seq - 1)
                ffn_stage(l)
            nxt = (order[i + 1][0], order[i + 1][1]) if i + 1 < len(order) else None
            store_load_stage(s, t0, 28 * depth, nxt)
        run_items()
        nsig = P.emit(nc)
        global _LAST_PE_LOG
        _LAST_PE_LOG = P.pe_log
    return nc, nsig


def make_prm(norm_mix, norm_ffn, ret_gn_gain, att_sinks, final_norm, depth):
    NP = prm_layout(depth)
    prm = np.zeros((128, NP), np.float32)
    for l in range(depth):
        prm[:, 28 * l + 0:28 * l + 8] = np.asarray(norm_mix[l], np.float32).reshape(8, 128).T
        prm[:, 28 * l + 8:28 * l + 16] = np.asarray(norm_ffn[l], np.float32).reshape(8, 128).T
        prm[:, 28 * l + 16:28 * l + 24] = np.asarray(ret_gn_gain[l], np.float32).reshape(8, 128).T
        sk = np.asarray(att_sinks[l], np.float32).reshape(4, 2)
        prm[0:64, 28 * l + 24:28 * l + 28] = sk[:, 0][None, :]
        prm[64:128, 28 * l + 24:28 * l + 28] = sk[:, 1][None, :]
    prm[:, 28 * depth:28 * depth + 8] = np.asarray(final_norm, np.float32).reshape(8, 128).T
    return prm


_CACHE = {}
_LAST_PE_LOG = None


def run(x, norm_mix, w_in, att_sinks, ret_gn_gain, w_att_o, w_ret_o, w_out, norm_ffn, w_gate, w_up, w_down,
        final_norm, n_cores=8, runner=None):
    x = np.ascontiguousarray(np.asarray(x, np.float32))
    depth = int(np.asarray(w_in).shape[0])
    batch, seq, _ = x.shape
    nseq = batch // n_cores
    key = (nseq, seq, depth)
    if key not in _CACHE:
        _CACHE[key] = build(nseq, seq, depth)
    nc, _ = _CACHE[key]
    c32, cbf, _ = make_consts()
    prm = make_prm(norm_mix, norm_ffn, ret_gn_gain, att_sinks, final_norm, depth)
    shared = dict(
        w_in=np.ascontiguousarray(np.asarray(w_in, np.float32)),
        w_att_o=np.ascontiguousarray(np.asarray(w_att_o, np.float32)),
        w_ret_o=np.ascontiguousarray(np.asarray(w_ret_o, np.float32)),
        w_out=np.ascontiguousarray(np.asarray(w_out, np.float32)),
        w_gate=np.ascontiguousarray(np.asarray(w_gate, np.float32)),
        w_up=np.ascontiguousarray(np.asarray(w_up, np.float32)),
        w_down=np.ascontiguousarray(np.asarray(w_down, np.float32)),
        c32=c32, cbf=cbf, prm=prm)
    in_maps = []
    for i in range(n_cores):
        m = dict(shared)
        m["x"] = np.ascontiguousarray(x[i * nseq:(i + 1) * nseq])
        in_maps.append(m)
    if runner is None:
        res = run_bass_kernel_spmd(nc, in_maps, core_ids=list(range(n_cores))).results
    else:
        res = runner(nc, in_maps)
    return np.concatenate([np.asarray(r["y"], np.float32) for r in res], axis=0)


def kernel(x, norm_mix, w_in, att_sinks, ret_gn_gain, w_att_o, w_ret_o, w_out, norm_ffn, w_gate, w_up, w_down,
           final_norm):
    return run(x, norm_mix, w_in, att_sinks, ret_gn_gain, w_att_o, w_ret_o, w_out, norm_ffn, w_gate, w_up, w_down,
               final_norm, n_cores=8)
```

```python
from contextlib import ExitStack
import numpy as np
import concourse.bass as bass
import concourse.mybir as mybir
from concourse.bass_utils import run_bass_kernel_spmd

F32 = mybir.dt.float32
BF16 = mybir.dt.bfloat16
AF = mybir.ActivationFunctionType
ALU = mybir.AluOpType

ENGS = ("pe", "act", "dve", "pool", "sp")
SEM_CAP = 30000


class Op:
    __slots__ = ("eng", "fn", "idx", "waits", "signal", "tick", "slot", "count", "clock", "dclock", "tag")


class Prog:
    def __init__(self):
        self.streams = {e: [] for e in ENGS}
        self.last_w = {}
        self.readers = {}
        self.clock = {e: {} for e in ENGS}
        self.dclock = {e: {} for e in ENGS}
        self.slot_count = {}
        self.slot_last = {}
        self.out_dmas = []
        self.last_acc = {}
        self.tag = ""
        self.pe_log = []

    def add(self, eng, fn, reads=(), writes=(), slot=None, is_out=False):
        excl = [k for k in list(reads) + list(writes) if isinstance(k, tuple) and k[0] == "ps"]
        op = Op()
        op.eng, op.fn = eng, fn
        op.tag = self.tag
        st = self.streams[eng]
        op.idx = len(st)
        op.signal = False
        op.tick = None
        op.slot = slot
        op.waits = []
        deps = []
        seen = set()

        def push(d):
            if d is not None and id(d) not in seen:
                seen.add(id(d))
                deps.append(d)

        for k in reads:
            push(self.last_w.get(k))
        for k in writes:
            push(self.last_w.get(k))
            for r in self.readers.get(k, ()):
                push(r)
        if slot is not None:
            push(self.slot_last.get(slot))
        for k in excl:
            for e2, d in self.last_acc.setdefault(k, {}).items():
                if e2 != eng:
                    push(d)
        ck = self.clock[eng]
        dk = self.dclock[eng]
        for d in deps:
            if d.slot is not None:
                if dk.get(d.slot, 0) >= d.count:
                    continue
                op.waits.append(d)
                dk[d.slot] = d.count
            else:
                if d.eng == eng and eng == "pe":
                    continue
                if ck.get(d.eng, 0) >= d.idx + 1:
                    continue
                op.waits.append(d)
                d.signal = True
                ck[d.eng] = d.idx + 1
            for e2, v in d.clock.items():
                if ck.get(e2, 0) < v:
                    ck[e2] = v
            for s2, v in d.dclock.items():
                if dk.get(s2, 0) < v:
                    dk[s2] = v
        if slot is not None:
            c = self.slot_count.get(slot, 0) + 1
            self.slot_count[slot] = c
            op.count = c
            self.slot_last[slot] = op
            op.clock = {e: v for e, v in ck.items() if e != eng}
            op.dclock = dict(dk)
            if is_out:
                self.out_dmas.append(op)
        else:
            op.count = 0
            op.clock = dict(ck)
            op.dclock = dict(dk)
        for k in excl:
            self.last_acc[k][eng] = op
        for k in reads:
            self.readers.setdefault(k, []).append(op)
        for k in writes:
            self.last_w[k] = op
            self.readers[k] = []
        st.append(op)
        return op

    def emit(self, nc):
        nsig = {}
        for e in ENGS:
            n = 0
            for op in self.streams[e]:
                if op.slot is None and op.signal:
                    n += 1
                    op.tick = n
            nsig[e] = n
        with ExitStack() as es:
            esem = {}
            for e in ENGS:
                k = max(1, (nsig[e] + SEM_CAP - 1) // SEM_CAP)
                esem[e] = [es.enter_context(nc.semaphore(f"s_{e}_{i}")) for i in range(k)]
            ssem = {}
            for i, s in enumerate(self.slot_count):
                ssem[s] = es.enter_context(nc.semaphore(f"d_{i}"))
            block = es.enter_context(nc.Block())

            def run(e, eng):
                for op in self.streams[e]:
                    for d in op.waits:
                        if d.slot is not None:
                            eng.wait_ge(ssem[d.slot], 16 * d.count)
                        else:
                            t = d.tick - 1
                            eng.wait_ge(esem[d.eng][t // SEM_CAP], t % SEM_CAP + 1)
                    if e == "pe" and self.pe_log is not None:
                        cnt = [0]

                        class _Px:
                            def matmul(_s, *a, **k):
                                cnt[0] += 1
                                return eng.matmul(*a, **k)

                            def transpose(_s, *a, **k):
                                cnt[0] += 1
                                return eng.transpose(*a, **k)
                        ins = op.fn(_Px())
                        self.pe_log.append((op.tag, cnt[0]))
                    else:
                        ins = op.fn(eng)
                    if op.slot is not None:
                        ins.then_inc(ssem[op.slot], 16)
                    elif op.signal:
                        t = op.tick - 1
                        ins.then_inc(esem[e][t // SEM_CAP], 1)
                if e == "sp":
                    last = {}
                    for d in self.out_dmas:
                        last[d.slot] = max(last.get(d.slot, 0), d.count)
                    for s, c in last.items():
                        eng.wait_ge(ssem[s], 16 * c)

            @block.tensor
            def _(eng):
                run("pe", eng)

            @block.scalar
            def _(eng):
                run("act", eng)

            @block.vector
            def _(eng):
                run("dve", eng)

            @block.gpsimd
            def _(eng):
                run("pool", eng)

            @block.sync
            def _(eng):
                run("sp", eng)
        return nsig


D = 1024
DFF = 2816
NFF = 22
MT = 1024
NBLK = 8
EPS = 1e-6
O_AQ, O_AK, O_AV, O_RQ, O_RK, O_RV, O_RG, O_GA, O_GR = 0, 512, 640, 768, 1280, 1792, 2816, 3840, 4864
NCH = 29
C_ID, C_ONE, C_DEC, C_DK, C_DQ, C32_N = 0, 128, 256, 768, 1280, 1792
B_ONESN, B_ONES256, B_OPAD, B_E, B_ID, CBF_N = 0, 128, 256, 512, 2560, 2688


def make_consts():
    c32 = np.zeros((128, C32_N), np.float32)
    c32[:, C_ID:C_ID + 128] = np.eye(128, dtype=np.float32)
    c32[:, C_ONE:C_ONE + 128] = 1.0
    log_g = np.log(1.0 - np.exp2(-5.0 - np.arange(4, dtype=np.float64)))
    j = np.arange(128)[:, None].astype(np.float64)
    i = np.arange(128)[None, :].astype(np.float64)
    for h in range(4):
        dec = np.where(i >= j, np.exp(log_g[h] * np.maximum(i - j, 0.0)), 0.0) * (128.0 ** -0.5)
        c32[:, C_DEC + h * 128:C_DEC + (h + 1) * 128] = dec
        c32[:, C_DK + h * 128:C_DK + (h + 1) * 128] = (np.exp(log_g[h] * (127.0 - j)) * (128.0 ** -0.5))
        c32[:, C_DQ + h * 128:C_DQ + (h + 1) * 128] = np.exp(log_g[h] * (i + 1.0))
    gC = [float(np.exp(log_g[h] * 128.0)) for h in range(4)]
    cbf = np.zeros((128, CBF_N), np.float32)
    cbf[:, B_ONESN:B_ONESN + 128] = 1.0 / 1024
    cbf[:, B_ONES256:B_ONES256 + 128] = 1.0 / 256
    cbf[:, B_OPAD:B_OPAD + 64] = 1.0
    cbf[:, B_OPAD + 128 + 64:B_OPAD + 256] = 1.0
    cbf[:, B_ID:B_ID + 128] = np.eye(128, dtype=np.float32)
    slopes = np.exp2(-8.0 * (np.arange(8, dtype=np.float64) + 1.0) / 8)
    for g in range(2):
        for half in range(2):
            for p in range(2):
                for cc in range(2):
                    hd = 4 * g + 2 * cc + p
                    dist = (i + 128.0 - j) if half == 0 else (i - j)
                    val = np.where((dist >= 0) & (dist < 128), np.exp(-slopes[hd] * dist), 0.0)
                    o = B_E + g * 1024 + half * 512 + p * 256 + cc * 128
                    cbf[:, o:o + 128] = val
    return c32, cbf, gC


def prm_layout(depth):
    return 28 * depth + 8


def build(nseq, seq, depth):
    assert seq % MT == 0
    mt_per_seq = seq // MT
    nc = bass.Bass("TRN2", target_bir_lowering=False)
    x_d = nc.dram_tensor("x", [nseq, seq, D], F32, kind="ExternalInput").ap()
    w_in = nc.dram_tensor("w_in", [depth, D, 5888], F32, kind="ExternalInput").ap()
    w_ao = nc.dram_tensor("w_att_o", [depth, 512, D], F32, kind="ExternalInput").ap()
    w_ro = nc.dram_tensor("w_ret_o", [depth, D, D], F32, kind="ExternalInput").ap()
    w_o = nc.dram_tensor("w_out", [depth, D, D], F32, kind="ExternalInput").ap()
    w_g = nc.dram_tensor("w_gate", [depth, D, DFF], F32, kind="ExternalInput").ap()
    w_u = nc.dram_tensor("w_up", [depth, D, DFF], F32, kind="ExternalInput").ap()
    w_d = nc.dram_tensor("w_down", [depth, DFF, D], F32, kind="ExternalInput").ap()
    c32_d = nc.dram_tensor("c32", [128, C32_N], F32, kind="ExternalInput").ap()
    cbf_d = nc.dram_tensor("cbf", [128, CBF_N], F32, kind="ExternalInput").ap()
    NP = prm_layout(depth)
    prm_d = nc.dram_tensor("prm", [128, NP], F32, kind="ExternalInput").ap()
    y_d = nc.dram_tensor("y", [nseq, seq, D], F32, kind="ExternalOutput").ap()
    _, _, gC = make_consts()

    P = Prog()
    with ExitStack() as es:
        def sb(name, shape, dt):
            return es.enter_context(nc.sbuf_tensor(name, shape, dt))

        xT = sb("xT", [128, 8, MT], F32)
        hT = sb("hT", [128, 8, MT], BF16)
        A = sb("arena", [128, NCH * 1024], BF16)
        NBUF = 5
        wbuf = [sb(f"wb{i}", [128, 4096], BF16) for i in range(NBUF)]
        c32 = sb("c32s", [128, C32_N], F32)
        cbf = sb("cbfs", [128, CBF_N], BF16)
        prm = sb("prms", [128, NP], F32)
        esink = sb("esink", [128, 4 * depth], F32)
        epsb = sb("epsb", [128, 1], F32)
        vpad = sb("vpad", [128, 9 * 512], BF16)
        vcarry = [sb(f"vcar{l}", [128, 512], BF16) for l in range(depth)]
        akcarry = [sb(f"akcar{l}", [128, 4, 128], BF16) for l in range(depth)]
        state = [sb(f"state{l}", [128, 1024], F32) for l in range(depth)]
        state_bf = sb("state_bf", [128, 1024], BF16)
        stg = [A[:, i * 2048:(i + 1) * 2048].bitcast(F32) for i in range(2)]
        sqt = [sb(f"sqt{i}", [128, 512], BF16) for i in range(2)]
        rinv = sb("rinv", [128, 512], F32)
        etmp2 = sb("etmp2", [128, 1024], F32)
        gtmp = [etmp2[:, i * 512:(i + 1) * 512] for i in range(2)]
        ST2 = [sb(f"ST{i}", [128, 512], BF16) for i in range(2)]
        qd2 = [sb(f"qdT{i}", [128, 512], BF16) for i in range(2)]
        etmp = sb("etmp", [128, 1024], F32)
        pT = [sb(f"pT{i}", [128, 1024], BF16) for i in range(2)]
        den = sb("den", [128, 512], F32)
        osq = [sb(f"osq{i}", [128, 512], BF16) for i in range(2)]
        pt = [es.enter_context(nc.psum_tensor(f"pt{i}", [128, 1024], F32)) for i in range(4)]

        def bank(i):
            return pt[i // 2][:, (i % 2) * 512:(i % 2) * 512 + 512]

        def bk(i):
            return ("ps", i)

        bank_ctr = [0]

        reserved = set()

        def nextbank():
            while True:
                b = bank_ctr[0] % 8
                bank_ctr[0] += 1
                if b not in reserved:
                    return b

        npre = {"banks": None, "pend": []}

        def npre_begin():
            b0, b1 = nextbank(), nextbank()
            reserved.update((b0, b1))
            npre["banks"] = [b0, b1]
            npre["pend"] = []
            npre["n"] = [0, 0]

        def npre_tile(c, tt):
            k = len(npre["pend"]) + sum(npre["n"])
            sq = sqt[k % 2]
            P.add("act", lambda e: e.activation(out=sq[:, :], in_=xT[:, c, tts(tt)], func=AF.Square),
                  reads=[("xT", c, tt)], writes=[("sqt", k % 2)])
            npre["pend"].append((sq, k % 2, tt))

        def npre_flush(keep):
            while len(npre["pend"]) > keep:
                sq, si, tt = npre["pend"].pop(0)
                b = npre["banks"][tt]
                i = npre["n"][tt]
                npre["n"][tt] += 1
                P.add("pe", lambda e, sq=sq, b=b, i=i: e.matmul(bank(b), cbf[:, B_ONESN:B_ONESN + 128], sq[:, :],
                                                                  start=(i == 0), stop=(i == 7)),
                      reads=[("sqt", si), "cbf"], writes=[bk(b)])

        def AR(ch, lo=0, hi=1024):
            return A[:, ch * 1024 + lo:ch * 1024 + hi]

        def akeys(lo, hi):
            return [("ar", g) for g in range(lo // 512, (hi - 1) // 512 + 1)]

        def ark(ch, lo=0, hi=1024):
            return akeys(ch * 1024 + lo, ch * 1024 + hi)

        def AR3(ch0, n, lo, hi):
            return A[:, ch0 * 1024:(ch0 + n) * 1024].rearrange("p (c t) -> p c t", c=n)[:, :, lo:hi]

        def ark3(ch0, n, lo, hi):
            ks = []
            for c in range(n):
                ks += ark(ch0 + c, lo, hi)
            return ks

        KPB = 8 * 1024

        def KP(q, lo, hi):
            return A[:, KPB + q * 1152 + lo:KPB + q * 1152 + hi]

        def kpk(q, lo, hi):
            return akeys(KPB + q * 1152 + lo, KPB + q * 1152 + hi)

        def tts(tt):
            return slice(tt * 512, tt * 512 + 512)

        eng_rr = [0]

        def evac_eng():
            eng_rr[0] += 1
            return "act" if eng_rr[0] % 2 else "dve"

        def copy_op(eng, out, in_, reads, writes, scale=None):
            if eng == "act":
                if scale is None:
                    P.add("act", lambda e: e.activation(out=out, in_=in_, func=AF.Copy), reads=reads, writes=writes)
                else:
                    P.add("act", lambda e: e.activation(out=out, in_=in_, func=AF.Copy, scale=scale),
                          reads=reads, writes=writes)
            else:
                assert scale is None
                P.add("dve", lambda e: e.tensor_copy(out=out, in_=in_), reads=reads, writes=writes)

        def mm_group(out_ap, pairs, reads, bkeys):
            def f(e):
                n = len(pairs)
                ins = None
                for i, (l, r) in enumerate(pairs):
                    ins = e.matmul(out_ap, l, r, start=(i == 0), stop=(i == n - 1))
                return ins
            P.add("pe", f, reads=reads, writes=bkeys)

        def mm_multi(groups, reads, bkeys):
            def f(e):
                ins = None
                for out_ap, pairs in groups:
                    n = len(pairs)
                    for i, (l, r) in enumerate(pairs):
                        ins = e.matmul(out_ap, l, r, start=(i == 0), stop=(i == n - 1))
                return ins
            P.add("pe", f, reads=reads, writes=bkeys)

        hkeys = {tt: [("hT", c, tt) for c in range(8)] for tt in range(2)}

        P.add("sp", lambda e: e.dma_start(out=c32[:, :], in_=c32_d[:, :]), writes=["c32"], slot="c32")
        P.add("sp", lambda e: e.dma_start(out=prm[:, :], in_=prm_d[:, :]), writes=["prm"], slot="prm")
        P.add("pool", lambda e: e.dma_start(out=cbf[:, :], in_=cbf_d[:, :]), writes=["cbf"], slot="cbf")
        P.add("dve", lambda e: e.memset(epsb[:, :], EPS), writes=["epsb"])
        P.add("dve", lambda e: e.memset(vpad[:, :], 0.0), writes=["vpad"])
        for l in range(depth):
            P.add("act", lambda e, l=l: e.activation(out=esink[:, 4 * l:4 * l + 4],
                                                     in_=prm[:, 28 * l + 24:28 * l + 28], func=AF.Exp),
                  reads=["prm"], writes=[("esink", l)])

        units = []

        def unit_simple(src, KC, ncols):
            return [(0, KC, ncols, 0, ncols, src.rearrange("(k p) n -> p k n", p=128))]

        items = []

        loaded = [0]
        all_units = []

        def emit_load(ui):
            pieces = all_units[ui]
            j = ui % NBUF
            for pi, (dlo, KC, ncols, clo, cn, src) in enumerate(pieces):
                dst = wbuf[j][:, dlo:dlo + KC * ncols].rearrange("p (k n) -> p k n", k=KC)[:, :, clo:clo + cn]
                P.add("pool", lambda e, dst=dst, src=src: e.dma_start(out=dst, in_=src),
                      writes=[("w", j)], slot=("w", j, pi))

        def run_items():
            base = 0
            for us, _ in items:
                all_units.extend(us)
            ui = 0
            for us, fn in items:
                need = min(len(all_units), ui + NBUF)
                while loaded[0] < need:
                    emit_load(loaded[0])
                    loaded[0] += 1
                views = []
                for k, u in enumerate(us):
                    j = (ui + k) % NBUF
                    _, KC, ncols, _, _, _ = u[0]
                    views.append((wbuf[j][:, 0:KC * ncols].rearrange("p (k n) -> p k n", k=KC), ("w", j)))
                P.tag = getattr(fn, "__name__", "?")
                fn(views)
                ui += len(us)

        def norm_stage(gcol, final=False, pre=False):
            def f_norm(_):
                if pre:
                    npre_flush(0)
                    assert npre["n"] == [8, 8]
                for tt in range(2):
                    b = npre["banks"][tt] if pre else nextbank()
                    for c in range(0 if pre else 8):
                        s = sqt[c % 2]
                        P.add("act", lambda e, s=s, c=c, tt=tt: e.activation(out=s[:, :], in_=xT[:, c, tts(tt)],
                                                                             func=AF.Square),
                              reads=[("xT", c, tt)], writes=[("sqt", c % 2)])
                        P.add("pe", lambda e, s=s, c=c, b=b: e.matmul(bank(b), cbf[:, B_ONESN:B_ONESN + 128], s[:, :],
                                                                      start=(c == 0), stop=(c == 7)),
                              reads=[("sqt", c % 2), "cbf"], writes=[bk(b)])
                    rb, rbk = (rinv, "rinv") if tt == 0 else (den, "den")
                    P.add("act", lambda e, b=b, rb=rb: e.activation(out=rb[:, :], in_=bank(b), func=AF.Ln,
                                                                    bias=epsb[:, 0:1]),
                          reads=[bk(b), "epsb"], writes=[rbk])
                    P.add("act", lambda e, rb=rb: e.activation(out=rb[:, :], in_=rb[:, :], func=AF.Exp, scale=-0.5),
                          reads=[rbk], writes=[rbk])
                    if pre:
                        reserved.discard(b)
                    for c in range(8):
                        if final:
                            P.add("dve", lambda e, c=c, tt=tt, rb=rb: e.scalar_tensor_tensor(
                                out=xT[:, c, tts(tt)], in0=xT[:, c, tts(tt)], scalar=prm[:, gcol + c:gcol + c + 1],
                                in1=rb[:, :], op0=ALU.mult, op1=ALU.mult),
                                reads=[("xT", c, tt), "prm", rbk], writes=[("xT", c, tt)])
                        else:
                            P.add("dve", lambda e, c=c, tt=tt, rb=rb: e.scalar_tensor_tensor(
                                out=hT[:, c, tts(tt)], in0=xT[:, c, tts(tt)], scalar=prm[:, gcol + c:gcol + c + 1],
                                in1=rb[:, :], op0=ALU.mult, op1=ALU.mult),
                                reads=[("xT", c, tt), "prm", rbk], writes=[("hT", c, tt)])
            items.append(([], f_norm))

        def fm_proj(wv, wk, ci, tt, KC, src, srckeys, b=None):
            if b is None:
                b = nextbank()
            mm_group(bank(b), [(wv[:, k, ci * 128:(ci + 1) * 128], src(k, tt)) for k in range(KC)],
                     reads=[wk] + srckeys, bkeys=[bk(b)])
            return b

        def h_src(k, tt):
            return hT[:, k, tts(tt)]

        def mixer_stage(l, first_mt, last_mt):
            wl = w_in[l]
            pb = 28 * l

            def f_rq(views):
                (wv, wk), = views
                for tt in range(2):
                    for h in range(4):
                        b = fm_proj(wv, wk, h, tt, 8, h_src, hkeys[tt])
                        copy_op(evac_eng(), AR(h, tt * 512, tt * 512 + 512), bank(b), [bk(b)], ark(h, tt * 512, tt * 512 + 512))
            items.append(([unit_simple(wl[:, O_RQ:O_RQ + 512], 8, 512)], f_rq))

            def f_rk(views):
                (wv, wk), = views
                for h in range(4):
                    for tt in range(2):
                        b = fm_proj(wv, wk, h, tt, 8, h_src, hkeys[tt])
                        copy_op(evac_eng(), AR(4 + h, tt * 512, tt * 512 + 512), bank(b), [bk(b)],
                                ark(4 + h, tt * 512, tt * 512 + 512))
            items.append(([unit_simple(wl[:, O_RK:O_RK + 512], 8, 512)], f_rk))

            def rk_tm_transposes():
                for blk in range(NBLK):
                    b = nextbank()
                    mm_multi([(bank(b)[:, h * 128:(h + 1) * 128],
                               [(AR(4 + h, blk * 128, blk * 128 + 128), cbf[:, B_ID:B_ID + 128])]) for h in range(4)],
                             reads=ark3(4, 4, blk * 128, blk * 128 + 128) + ["cbf"], bkeys=[bk(b)])
                    lo = (blk % 2) * 512
                    P.add("dve", lambda e, b=b, blk=blk, lo=lo: e.tensor_tensor(
                        out=AR(8 + blk // 2, lo, lo + 512), in0=bank(b), in1=c32[:, C_DK:C_DK + 512], op=ALU.mult),
                        reads=[bk(b), "c32"], writes=ark(8 + blk // 2, lo, lo + 512))

            for half in range(2):
                def f_rv(views, half=half):
                    (wv, wk), = views
                    for blk in range(NBLK):
                        b = nextbank()
                        tt = blk // 4
                        mm_group(bank(b), [(hT[:, k, blk * 128:(blk + 1) * 128], wv[:, k, :]) for k in range(8)],
                                 reads=[wk] + hkeys[tt], bkeys=[bk(b)])
                        copy_op(evac_eng(), AR(12 + blk, half * 512, half * 512 + 512), bank(b), [bk(b)],
                                ark(12 + blk, half * 512, half * 512 + 512))
                    if half == 1:
                        rk_tm_transposes()
                items.append(([unit_simple(wl[:, O_RV + half * 512:O_RV + half * 512 + 512], 8, 512)], f_rv))

            def f_ret(_):
                if not first_mt:
                    P.add("act", lambda e: e.activation(out=state_bf[:, :], in_=state[l][:, :], func=AF.Copy),
                          reads=[("state", l)], writes=["state_bf"])

                def isfirst(blk):
                    return first_mt and blk == 0

                def islast(blk):
                    return last_mt and blk == NBLK - 1

                def pe_front(blk):
                    sb_ = blk % 2
                    mm_multi([(bank(sb_)[:, h * 128:(h + 1) * 128], [(AR(4 + h, blk * 128, blk * 128 + 128),
                                                                      AR(h, blk * 128, blk * 128 + 128))]) for h in range(4)],
                             reads=ark3(0, 8, blk * 128, blk * 128 + 128), bkeys=[bk(sb_)])
                    if not islast(blk):
                        lo = (blk % 2) * 512
                        su = pt[2 + blk % 2]
                        mm_multi([(su[:, h * 256:(h + 1) * 256],
                                   [(AR(8 + blk // 2, lo + h * 128, lo + h * 128 + 128), AR(12 + blk, h * 256, h * 256 + 256))])
                                  for h in range(4)],
                                 reads=ark(8 + blk // 2, lo, lo + 512) + ark(12 + blk),
                                 bkeys=[bk(4 + 2 * (blk % 2)), bk(5 + 2 * (blk % 2))])

                def dve_front(blk):
                    sb_ = blk % 2
                    STb = ST2[blk % 2]
                    P.add("dve", lambda e: e.tensor_tensor(out=STb[:, :], in0=bank(sb_), in1=c32[:, C_DEC:C_DEC + 512],
                                                           op=ALU.mult),
                          reads=[bk(sb_), "c32"], writes=[("ST", blk % 2)])
                    if not isfirst(blk):
                        qb = qd2[blk % 2]
                        P.add("dve", lambda e: e.tensor_tensor(
                            out=qb[:, :].rearrange("p (h i) -> p h i", h=4), in0=AR3(0, 4, blk * 128, blk * 128 + 128),
                            in1=c32[:, C_DQ:C_DQ + 512].rearrange("p (h i) -> p h i", h=4), op=ALU.mult),
                            reads=ark3(0, 4, blk * 128, blk * 128 + 128) + ["c32"], writes=[("qdT", blk % 2)])

                def back(blk):
                    first = isfirst(blk)
                    STb = ST2[blk % 2]
                    qb = qd2[blk % 2]
                    groups = []
                    for h in range(4):
                        for jj in range(2):
                            c = 2 * h + jj
                            pairs = [(AR(12 + blk, h * 256 + jj * 128, h * 256 + jj * 128 + 128), STb[:, h * 128:(h + 1) * 128])]
                            if not first:
                                pairs.append((state_bf[:, h * 256 + jj * 128:h * 256 + jj * 128 + 128],
                                              qb[:, h * 128:(h + 1) * 128]))
                            groups.append((pt[1][:, c * 128:(c + 1) * 128], pairs))
                    mm_multi(groups, reads=ark(12 + blk) + [("ST", blk % 2)] + ([] if first else ["state_bf", ("qdT", blk % 2)]),
                             bkeys=[bk(2), bk(3)])
                    copy_op("act", AR3(21, 8, blk * 128, blk * 128 + 128), pt[1][:, :].rearrange("p (c t) -> p c t", c=8),
                            [bk(2), bk(3)], ark3(21, 8, blk * 128, blk * 128 + 128))
                    if not islast(blk):
                        su = pt[2 + blk % 2]
                        sk = [bk(4 + 2 * (blk % 2)), bk(5 + 2 * (blk % 2))]
                        if first:
                            P.add("dve", lambda e: e.tensor_copy(out=state[l][:, :], in_=su[:, :]),
                                  reads=sk, writes=[("state", l)])
                        else:
                            for h in range(4):
                                P.add("dve", lambda e, h=h: e.scalar_tensor_tensor(
                                    out=state[l][:, h * 256:(h + 1) * 256], in0=state[l][:, h * 256:(h + 1) * 256],
                                    scalar=gC[h], in1=su[:, h * 256:(h + 1) * 256], op0=ALU.mult, op1=ALU.add),
                                    reads=sk + [("state", l)], writes=[("state", l)])
                        P.add("act", lambda e: e.activation(out=state_bf[:, :], in_=state[l][:, :], func=AF.Copy),
                              reads=[("state", l)], writes=["state_bf"])

                pe_front(0)
                for blk in range(NBLK):
                    dve_front(blk)
                    if blk + 1 < NBLK:
                        pe_front(blk + 1)
                    back(blk)
            items.append(([], f_ret))

            akv_src = lambda c0, n: wl[:, c0:c0 + n].rearrange("(k p) n -> p k n", p=128)
            akv_unit = [(0, 8, 384, 0, 64, akv_src(O_AK, 64)), (0, 8, 384, 64, 64, akv_src(O_AK, 64)),
                        (0, 8, 384, 128, 64, akv_src(O_AK + 64, 64)), (0, 8, 384, 192, 64, akv_src(O_AK + 64, 64)),
                        (0, 8, 384, 256, 128, akv_src(O_AV, 128))]

            def f_gn(views):
                (wq, wqk), (wkv, wkvk) = views
                fillers = []
                for c in range(4):
                    for tt in range(2):
                        def f_aq_tile(c=c, tt=tt):
                            lo, hi = tt * 512, tt * 512 + 512
                            b = fm_proj(wq, wqk, c, tt, 8, h_src, hkeys[tt])
                            return lambda: copy_op(evac_eng(), AR(c, lo, hi), bank(b), [bk(b)], ark(c, lo, hi))
                        fillers.append(f_aq_tile)

                def akv_prep():
                    for q in range(4):
                        p = q % 2
                        rows = slice(64, 128) if p == 0 else slice(0, 64)
                        P.add("dve", lambda e, q=q, rows=rows: e.memset(A[rows, KPB + q * 1152:KPB + (q + 1) * 1152], 0.0),
                              writes=kpk(q, 0, 1152))
                    if not first_mt:
                        P.add("dve", lambda e: e.tensor_copy(
                            out=A[:, KPB:KPB + 4 * 1152].rearrange("p (q t) -> p q t", q=4)[:, :, 0:128], in_=akcarry[l][:, :, :]),
                            reads=[("akcar", l)], writes=[k for q in range(4) for k in kpk(q, 0, 128)])
                        P.add("dve", lambda e: e.tensor_copy(out=vpad[:, 0:512], in_=vcarry[l][:, :]),
                              reads=[("vcar", l)], writes=["vpad"])

                for g in range(2):
                    for tt in range(2):
                        def f_k_tile(g=g, tt=tt):
                            b = fm_proj(wkv, wkvk, g, tt, 8, h_src, hkeys[tt])
                            lo = 128 + tt * 512

                            def ev():
                                P.add("act", lambda e: e.activation(out=A[0:64, KPB + (2 * g) * 1152 + lo:KPB + (2 * g) * 1152 + lo + 512],
                                                                    in_=bank(b)[0:64, :], func=AF.Copy),
                                      reads=[bk(b)], writes=kpk(2 * g, lo, lo + 512))
                                P.add("dve", lambda e: e.tensor_copy(
                                    out=A[64:128, KPB + (2 * g + 1) * 1152 + lo:KPB + (2 * g + 1) * 1152 + lo + 512], in_=bank(b)[64:128, :]),
                                    reads=[bk(b)], writes=kpk(2 * g + 1, lo, lo + 512))
                            return ev
                        fillers.append(f_k_tile)
                for bq in range(2):
                    def f_v_tile(bq=bq):
                        b = nextbank()
                        groups = []
                        for bi in range(4):
                            blk = bq * 4 + bi
                            groups.append((bank(b)[:, bi * 128:(bi + 1) * 128],
                                           [(hT[:, k, blk * 128:(blk + 1) * 128], wkv[:, k, 256:384]) for k in range(8)]))
                        mm_multi(groups, reads=[wkvk] + hkeys[bq], bkeys=[bk(b)])

                        def ev():
                            vv = vpad[:, :].rearrange("p (s q d) -> p s q d", s=9, q=4)
                            src4 = bank(b).rearrange("p (s d) -> p s d", s=4)
                            for g in range(2):
                                for p in range(2):
                                    dst = vv[:, 1 + bq * 4:1 + bq * 4 + 4, 2 * g + p, 64 * p:64 * p + 64]
                                    srcv = src4[:, :, g * 64:(g + 1) * 64]
                                    copy_op(evac_eng(), dst, srcv, [bk(b)], ["vpad"])
                        return ev
                    fillers.append(f_v_tile)

                pending = []

                def fill(n):
                    for _ in range(n):
                        if fillers:
                            pending.append(fillers.pop(0)())

                def drain():
                    while pending:
                        pending.pop(0)()

                akv_prep()
                slot = 0
                for h in range(4):
                    for tt in range(2):
                        lo, hi = tt * 512, tt * 512 + 512
                        bm, bq = nextbank(), nextbank()
                        for jj in range(2):
                            c = 21 + 2 * h + jj
                            P.add("act", lambda e, c=c, jj=jj, lo=lo, hi=hi: e.activation(out=osq[jj][:, :], in_=AR(c, lo, hi),
                                                                                          func=AF.Square),
                                  reads=ark(c, lo, hi), writes=[("osq", jj)])
                        mm_group(bank(bm), [(cbf[:, B_ONES256:B_ONES256 + 128], AR(21 + 2 * h + jj, lo, hi)) for jj in range(2)],
                                 reads=["cbf"] + ark(21 + 2 * h, lo, hi) + ark(22 + 2 * h, lo, hi), bkeys=[bk(bm)])
                        mm_group(bank(bq), [(cbf[:, B_ONES256:B_ONES256 + 128], osq[jj][:, :]) for jj in range(2)],
                                 reads=["cbf", ("osq", 0), ("osq", 1)], bkeys=[bk(bq)])
                        fill(2 if slot % 4 != 3 else 1)
                        slot += 1
                        P.add("act", lambda e, bm=bm: e.activation(out=etmp[:, 0:512], in_=bank(bm), func=AF.Square),
                              reads=[bk(bm)], writes=[("etmp", 0)])
                        P.add("dve", lambda e, bq=bq: e.tensor_tensor(out=etmp[:, 512:1024], in0=bank(bq), in1=etmp[:, 0:512],
                                                                      op=ALU.subtract),
                              reads=[bk(bq), ("etmp", 0)], writes=[("etmp", 1)])
                        P.add("act", lambda e: e.activation(out=etmp[:, 512:1024], in_=etmp[:, 512:1024], func=AF.Ln,
                                                            bias=epsb[:, 0:1]),
                              reads=[("etmp", 1), "epsb"], writes=[("etmp", 1)])
                        P.add("act", lambda e: e.activation(out=etmp[:, 512:1024], in_=etmp[:, 512:1024], func=AF.Exp,
                                                            scale=-0.5),
                              reads=[("etmp", 1)], writes=[("etmp", 1)])
                        for jj in range(2):
                            c = 21 + 2 * h + jj
                            gcol = pb + 16 + 2 * h + jj
                            gt = gtmp[jj]
                            P.add("dve", lambda e, c=c, gt=gt, bm=bm, lo=lo, hi=hi: e.tensor_tensor(
                                out=gt[:, :], in0=AR(c, lo, hi), in1=bank(bm), op=ALU.subtract),
                                reads=ark(c, lo, hi) + [bk(bm)], writes=[("gtmp", jj)])
                            P.add("dve", lambda e, c=c, gt=gt, gcol=gcol, lo=lo, hi=hi: e.scalar_tensor_tensor(
                                out=AR(c, lo, hi), in0=gt[:, :], scalar=prm[:, gcol:gcol + 1], in1=etmp[:, 512:1024],
                                op0=ALU.mult, op1=ALU.mult),
                                reads=[("gtmp", jj), "prm", ("etmp", 1)], writes=ark(c, lo, hi))
                        drain()
                while fillers:
                    fill(1)
                    drain()
            items.append(([unit_simple(wl[:, O_AQ:O_AQ + 512], 8, 512), akv_unit], f_gn))

            for u in range(2):
                def f_rg(views, u=u):
                    (wv, wk), = views
                    for ci in range(4):
                        c = u * 4 + ci
                        for tt in range(2):
                            lo, hi = tt * 512, tt * 512 + 512
                            b = fm_proj(wv, wk, ci, tt, 8, h_src, hkeys[tt])
                            gi = (c * 2 + tt) % 2
                            gt = gtmp[gi]
                            P.add("act", lambda e, b=b, gt=gt: e.activation(out=gt[:, :], in_=bank(b), func=AF.Silu),
                                  reads=[bk(b)], writes=[("gtmp", gi)])
                            P.add("dve", lambda e, c=c, gt=gt, lo=lo, hi=hi: e.tensor_tensor(
                                out=AR(21 + c, lo, hi), in0=AR(21 + c, lo, hi), in1=gt[:, :], op=ALU.mult),
                                reads=ark(21 + c, lo, hi) + [("gtmp", gi)], writes=ark(21 + c, lo, hi))
                items.append(([unit_simple(wl[:, O_RG + u * 512:O_RG + u * 512 + 512], 8, 512)], f_rg))

            def f_att(_):
                vv = vpad[:, :].rearrange("p (s q d) -> p s q d", s=9, q=4)

                def halves_of(blk):
                    return [1] if (first_mt and blk == 0) else [0, 1]

                def S(blk, g):
                    groups = []
                    rk_ = []
                    for half in halves_of(blk):
                        for p in range(2):
                            q = 2 * g + p
                            kc = (blk + half) * 128
                            o = half * 512 + p * 256
                            groups.append((pt[g][:, o:o + 256],
                                           [(KP(q, kc, kc + 128), AR3(2 * g, 2, blk * 128, blk * 128 + 128))]))
                            rk_ += kpk(q, kc, kc + 128)
                    mm_multi(groups, reads=rk_ + ark3(0, 4, blk * 128, blk * 128 + 128), bkeys=[bk(2 * g), bk(2 * g + 1)])

                def softmax(blk, g):
                    lo = 512 if (first_mt and blk == 0) else 0
                    eb = etmp if g == 0 else etmp2
                    ek = [("etmp", 0), ("etmp", 1)] if g == 0 else [("gtmp", 0), ("gtmp", 1)]
                    P.add("act", lambda e: e.activation(out=eb[:, lo:1024], in_=pt[g][:, lo:1024], func=AF.Exp, scale=0.125),
                          reads=[bk(2 * g), bk(2 * g + 1)], writes=ek)
                    pg = pT[g]
                    P.add("dve", lambda e: e.tensor_tensor(
                        out=pg[:, lo:1024], in0=eb[:, lo:1024], in1=cbf[:, B_E + g * 1024 + lo:B_E + g * 1024 + 1024],
                        op=ALU.mult),
                        reads=ek + ["cbf"], writes=[("pT", g)])

                def PV(blk, g):
                    pg = pT[g]
                    ob = pt[2 + blk % 2]
                    pv, dn = [], []
                    for half in halves_of(blk):
                        for p in range(2):
                            o = half * 512 + p * 256
                            pv.append((vv[:, blk + half, 2 * g + p, :], pg[:, o:o + 256]))
                            dn.append((cbf[:, B_OPAD + p * 128:B_OPAD + p * 128 + 128], pg[:, o:o + 256]))
                    mm_multi([(ob[:, g * 256:g * 256 + 256], pv), (ob[:, 512 + g * 256:512 + g * 256 + 256], dn)],
                             reads=["vpad", "cbf", ("pT", g)], bkeys=[bk(4 + 2 * (blk % 2)), bk(5 + 2 * (blk % 2))])

                def fin(blk):
                    ob = pt[2 + blk % 2]
                    kb = [bk(4 + 2 * (blk % 2)), bk(5 + 2 * (blk % 2))]
                    for c in range(4):
                        P.add("act", lambda e, c=c: e.activation(
                            out=den[:, c * 128:(c + 1) * 128], in_=ob[:, 512 + c * 128:512 + (c + 1) * 128], func=AF.Ln,
                            bias=esink[:, 4 * l + c:4 * l + c + 1]),
                            reads=kb + [("esink", l)], writes=["den"])
                    P.add("act", lambda e: e.activation(out=den[:, :], in_=den[:, :], func=AF.Exp, scale=-1.0),
                          reads=["den"], writes=["den"])
                    P.add("dve", lambda e: e.tensor_tensor(
                        out=AR3(4, 4, blk * 128, blk * 128 + 128), in0=ob[:, 0:512].rearrange("p (c t) -> p c t", c=4),
                        in1=den[:, :].rearrange("p (c t) -> p c t", c=4), op=ALU.mult),
                        reads=kb + ["den"], writes=ark3(4, 4, blk * 128, blk * 128 + 128))

                S(0, 0)
                S(0, 1)
                softmax(0, 0)
                softmax(0, 1)
                for blk in range(NBLK):
                    PV(blk, 0)
                    if blk + 1 < NBLK:
                        S(blk + 1, 0)
                    PV(blk, 1)
                    if blk + 1 < NBLK:
                        S(blk + 1, 1)
                        softmax(blk + 1, 0)
                        softmax(blk + 1, 1)
                    fin(blk)
                if not last_mt:
                    P.add("dve", lambda e: e.tensor_copy(
                        out=akcarry[l][:, :, :], in_=A[:, KPB:KPB + 4 * 1152].rearrange("p (q t) -> p q t", q=4)[:, :, 1024:1152]),
                        reads=[k for q in range(4) for k in kpk(q, 1024, 1152)], writes=[("akcar", l)])
                    P.add("dve", lambda e: e.tensor_copy(out=vcarry[l][:, :], in_=vpad[:, 8 * 512:9 * 512]),
                          reads=["vpad"], writes=[("vcar", l)])
            items.append(([], f_att))

            for u in range(2):
                def f_ao(views, u=u):
                    (wa, wak), (wg, wgk) = views
                    for ci in range(4):
                        c = u * 4 + ci
                        for tt in range(2):
                            lo, hi = tt * 512, tt * 512 + 512
                            ba = fm_proj(wa, wak, ci, tt, 4, lambda k, tt: AR(4 + k, tt * 512, tt * 512 + 512),
                                         ark3(4, 4, lo, hi))
                            bg = fm_proj(wg, wgk, ci, tt, 8, h_src, hkeys[tt])
                            gi = (c * 2 + tt) % 2
                            gt = gtmp[gi]
                            P.add("act", lambda e, bg=bg, gt=gt: e.activation(out=gt[:, :], in_=bank(bg), func=AF.Sigmoid),
                                  reads=[bk(bg)], writes=[("gtmp", gi)])
                            P.add("dve", lambda e, c=c, ba=ba, gt=gt, lo=lo, hi=hi: e.tensor_tensor(
                                out=AR(13 + c, lo, hi), in0=bank(ba), in1=gt[:, :], op=ALU.mult),
                                reads=[bk(ba), ("gtmp", gi)], writes=ark(13 + c, lo, hi))
                items.append(([unit_simple(w_ao[l][:, u * 512:u * 512 + 512], 4, 512),
                               unit_simple(wl[:, O_GA + u * 512:O_GA + u * 512 + 512], 8, 512)], f_ao))

            for u in range(2):
                def f_ro(views, u=u):
                    (wr, wrk), (wg, wgk) = views
                    for ci in range(4):
                        c = u * 4 + ci
                        for tt in range(2):
                            lo, hi = tt * 512, tt * 512 + 512
                            br = fm_proj(wr, wrk, ci, tt, 8, lambda k, tt: AR(21 + k, tt * 512, tt * 512 + 512),
                                         ark3(21, 8, lo, hi))
                            bg = fm_proj(wg, wgk, ci, tt, 8, h_src, hkeys[tt])
                            gi = (c * 2 + tt) % 2
                            gt = gtmp[gi]
                            P.add("act", lambda e, bg=bg, gt=gt: e.activation(out=gt[:, :], in_=bank(bg), func=AF.Sigmoid),
                                  reads=[bk(bg)], writes=[("gtmp", gi)])
                            P.add("dve", lambda e, br=br, gt=gt: e.tensor_tensor(
                                out=gt[:, :], in0=bank(br), in1=gt[:, :], op=ALU.mult),
                                reads=[bk(br), ("gtmp", gi)], writes=[("gtmp", gi)])
                            P.add("dve", lambda e, c=c, gt=gt, lo=lo, hi=hi: e.tensor_tensor(
                                out=AR(13 + c, lo, hi), in0=AR(13 + c, lo, hi), in1=gt[:, :], op=ALU.add),
                                reads=ark(13 + c, lo, hi) + [("gtmp", gi)], writes=ark(13 + c, lo, hi))
                items.append(([unit_simple(w_ro[l][:, u * 512:u * 512 + 512], 8, 512),
                               unit_simple(wl[:, O_GR + u * 512:O_GR + u * 512 + 512], 8, 512)], f_ro))

            for u in range(2):
                def f_o(views, u=u):
                    (wv, wk), = views
                    if u == 0:
                        npre_begin()
                    for ci in range(4):
                        c = u * 4 + ci
                        for tt in range(2):
                            lo, hi = tt * 512, tt * 512 + 512
                            b = fm_proj(wv, wk, ci, tt, 8, lambda k, tt: AR(13 + k, tt * 512, tt * 512 + 512),
                                        ark3(13, 8, lo, hi))
                            npre_flush(1)
                            P.add("dve", lambda e, c=c, b=b, tt=tt: e.tensor_tensor(
                                out=xT[:, c, tts(tt)], in0=xT[:, c, tts(tt)], in1=bank(b), op=ALU.add),
                                reads=[bk(b), ("xT", c, tt)], writes=[("xT", c, tt)])
                            npre_tile(c, tt)
                items.append(([unit_simple(w_o[l][:, u * 512:u * 512 + 512], 8, 512)], f_o))

        def ffn_stage(l):
            norm_stage(28 * l + 8, pre=True)
            for u in range(6):
                n = 512 if u < 5 else 256

                def f_gu(views, u=u, n=n):
                    (wg, wgk), (wu, wuk) = views
                    for tt in range(2):
                        for ci in range(n // 128):
                            c = u * 4 + ci
                            lo, hi = tt * 512, tt * 512 + 512
                            bg = fm_proj(wg, wgk, ci, tt, 8, h_src, hkeys[tt])
                            bu = fm_proj(wu, wuk, ci, tt, 8, h_src, hkeys[tt])
                            gi = (ci + tt) % 2
                            gt = gtmp[gi]
                            P.add("act", lambda e, bg=bg, gt=gt: e.activation(out=gt[:, :], in_=bank(bg), func=AF.Silu),
                                  reads=[bk(bg)], writes=[("gtmp", gi)])
                            P.add("dve", lambda e, c=c, bu=bu, gt=gt, lo=lo, hi=hi: e.tensor_tensor(
                                out=AR(c, lo, hi), in0=bank(bu), in1=gt[:, :], op=ALU.mult),
                                reads=[bk(bu), ("gtmp", gi)], writes=ark(c, lo, hi))
                items.append(([unit_simple(w_g[l][:, u * 512:u * 512 + n], 8, n),
                               unit_simple(w_u[l][:, u * 512:u * 512 + n], 8, n)], f_gu))
            for c in range(8):
                def f_dn(views, c=c):
                    (wv, wk), = views
                    if c == 0:
                        npre_begin()
                    for tt in range(2):
                        lo, hi = tt * 512, tt * 512 + 512
                        b = nextbank()
                        mm_group(bank(b), [(wv[:, k, :], AR(k, lo, hi)) for k in range(NFF)],
                                 reads=[wk] + ark3(0, NFF, lo, hi), bkeys=[bk(b)])
                        npre_flush(1)
                        P.add("dve", lambda e, c=c, b=b, tt=tt: e.tensor_tensor(
                            out=xT[:, c, tts(tt)], in0=xT[:, c, tts(tt)], in1=bank(b), op=ALU.add),
                            reads=[bk(b), ("xT", c, tt)], writes=[("xT", c, tt)])
                        npre_tile(c, tt)
                items.append(([unit_simple(w_d[l][:, c * 128:(c + 1) * 128], NFF, 128)], f_dn))

        stgL = [A[:, (22 + 2 * i) * 1024:(24 + 2 * i) * 1024].bitcast(F32) for i in range(2)]

        def ld_dma(s, t0, blk):
            sg = stgL[blk % 2]
            P.add("sp", lambda e: e.dma_start(out=sg[:, :], in_=x_d[s, t0 + blk * 128:t0 + blk * 128 + 128, :]),
                  writes=ark3(22 + 2 * (blk % 2), 2, 0, 1024), slot=("ld", blk % 2))

        def ld_xpose(blk):
            sg = stgL[blk % 2]
            tt = blk // 4
            for hf in range(2):
                b = nextbank()

                def f(e, hf=hf, b=b):
                    ins = None
                    for jx in range(4):
                        c = hf * 4 + jx
                        ins = e.transpose(bank(b)[:, jx * 128:(jx + 1) * 128], sg[:, c * 128:(c + 1) * 128],
                                          c32[:, C_ID:C_ID + 128])
                    return ins
                P.add("pe", f, reads=ark3(22 + 2 * (blk % 2), 2, 0, 1024) + ["c32"], writes=[bk(b)])
                copy_op(evac_eng(), xT[:, hf * 4:hf * 4 + 4, blk * 128:blk * 128 + 128],
                        bank(b).rearrange("p (c t) -> p c t", c=4), [bk(b)],
                        [("xT", c, tt) for c in range(hf * 4, hf * 4 + 4)])

        def st_block(s, t0, blk):
            sg = stg[blk % 2]
            tt = blk // 4
            for hf in range(2):
                b = nextbank()

                def f(e, hf=hf, b=b):
                    ins = None
                    for jx in range(4):
                        c = hf * 4 + jx
                        ins = e.transpose(bank(b)[:, jx * 128:(jx + 1) * 128], xT[:, c, blk * 128:blk * 128 + 128],
                                          c32[:, C_ID:C_ID + 128])
                    return ins
                P.add("pe", f, reads=[("xT", c, tt) for c in range(hf * 4, hf * 4 + 4)] + ["c32"], writes=[bk(b)])
                copy_op(evac_eng(), sg[:, hf * 512:hf * 512 + 512], bank(b), [bk(b)], ark(2 * (blk % 2) + hf))
            P.add("sp", lambda e: e.dma_start(out=y_d[s, t0 + blk * 128:t0 + blk * 128 + 128, :], in_=sg[:, :]),
                  reads=ark3(2 * (blk % 2), 2, 0, 1024), slot=("st", blk % 2), is_out=True)

        def load_stage(s, t0):
            def f_load(_):
                ld_dma(s, t0, 0)
                ld_dma(s, t0, 1)
                for blk in range(NBLK):
                    ld_xpose(blk)
                    if blk + 2 < NBLK:
                        ld_dma(s, t0, blk + 2)
            items.append(([], f_load))

        def store_load_stage(s, t0, gcol, nxt):
            if nxt is not None:
                def f_pre(_):
                    ld_dma(nxt[0], nxt[1], 0)
                    ld_dma(nxt[0], nxt[1], 1)
                items.append(([], f_pre))
            norm_stage(gcol, final=True, pre=True)

            def f_store(_):
                for blk in range(4):
                    st_block(s, t0, blk)
                for blk in range(4):
                    if nxt is not None:
                        ld_xpose(blk)
                        ld_dma(nxt[0], nxt[1], blk + 2)
                    st_block(s, t0, 4 + blk)
                if nxt is not None:
                    for blk in range(4, NBLK):
                        ld_xpose(blk)
                        if blk + 2 < NBLK:
                            ld_dma(nxt[0], nxt[1], blk + 2)
            items.append(([], f_store))

        order = [(s, m * MT, m) for s in range(nseq) for m in range(mt_per_seq)]
        load_stage(order[0][0], order[0][1])
        for i, (s, t0, m) in enumerate(order):
            for l in range(depth):
                norm_stage(28 * l, pre=(l > 0))
                mixer_stage(l, m == 0, m == mt_per_seq - 1)
                ffn_stage(l)
            nxt = (order[i + 1][0], order[i + 1][1]) if i + 1 < len(order) else None
            store_load_stage(s, t0, 28 * depth, nxt)
        run_items()
        nsig = P.emit(nc)
        global _LAST_PE_LOG
        _LAST_PE_LOG = P.pe_log
    return nc, nsig


def make_prm(norm_mix, norm_ffn, ret_gn_gain, att_sinks, final_norm, depth):
    NP = prm_layout(depth)
    prm = np.zeros((128, NP), np.float32)
    for l in range(depth):
        prm[:, 28 * l + 0:28 * l + 8] = np.asarray(norm_mix[l], np.float32).reshape(8, 128).T
        prm[:, 28 * l + 8:28 * l + 16] = np.asarray(norm_ffn[l], np.float32).reshape(8, 128).T
        prm[:, 28 * l + 16:28 * l + 24] = np.asarray(ret_gn_gain[l], np.float32).reshape(8, 128).T
        sk = np.asarray(att_sinks[l], np.float32).reshape(4, 2)
        prm[0:64, 28 * l + 24:28 * l + 28] = sk[:, 0][None, :]
        prm[64:128, 28 * l + 24:28 * l + 28] = sk[:, 1][None, :]
    prm[:, 28 * depth:28 * depth + 8] = np.asarray(final_norm, np.float32).reshape(8, 128).T
    return prm


_CACHE = {}
_LAST_PE_LOG = None


def run(x, norm_mix, w_in, att_sinks, ret_gn_gain, w_att_o, w_ret_o, w_out, norm_ffn, w_gate, w_up, w_down,
        final_norm, n_cores=8, runner=None):
    x = np.ascontiguousarray(np.asarray(x, np.float32))
    depth = int(np.asarray(w_in).shape[0])
    batch, seq, _ = x.shape
    nseq = batch // n_cores
    key = (nseq, seq, depth)
    if key not in _CACHE:
        _CACHE[key] = build(nseq, seq, depth)
    nc, _ = _CACHE[key]
    c32, cbf, _ = make_consts()
    prm = make_prm(norm_mix, norm_ffn, ret_gn_gain, att_sinks, final_norm, depth)
    shared = dict(
        w_in=np.ascontiguousarray(np.asarray(w_in, np.float32)),
        w_att_o=np.ascontiguousarray(np.asarray(w_att_o, np.float32)),
        w_ret_o=np.ascontiguousarray(np.asarray(w_ret_o, np.float32)),
        w_out=np.ascontiguousarray(np.asarray(w_out, np.float32)),
        w_gate=np.ascontiguousarray(np.asarray(w_gate, np.float32)),
        w_up=np.ascontiguousarray(np.asarray(w_up, np.float32)),
        w_down=np.ascontiguousarray(np.asarray(w_down, np.float32)),
        c32=c32, cbf=cbf, prm=prm)
    in_maps = []
    for i in range(n_cores):
        m = dict(shared)
        m["x"] = np.ascontiguousarray(x[i * nseq:(i + 1) * nseq])
        in_maps.append(m)
    if runner is None:
        res = run_bass_kernel_spmd(nc, in_maps, core_ids=list(range(n_cores))).results
    else:
        res = runner(nc, in_maps)
    return np.concatenate([np.asarray(r["y"], np.float32) for r in res], axis=0)


def kernel(x, norm_mix, w_in, att_sinks, ret_gn_gain, w_att_o, w_ret_o, w_out, norm_ffn, w_gate, w_up, w_down,
           final_norm):
    return run(x, norm_mix, w_in, att_sinks, ret_gn_gain, w_att_o, w_ret_o, w_out, norm_ffn, w_gate, w_up, w_down,
               final_norm, n_cores=8)
```

```python
from contextlib import ExitStack
import numpy as np
import concourse.bass as bass
import concourse.mybir as mybir
from concourse.bass_utils import run_bass_kernel_spmd

F32 = mybir.dt.float32
BF16 = mybir.dt.bfloat16
AF = mybir.ActivationFunctionType
ALU = mybir.AluOpType

ENGS = ("pe", "act", "dve", "pool", "sp")
SEM_CAP = 30000


class Op:
    __slots__ = ("eng", "fn", "idx", "waits", "signal", "tick", "slot", "count", "clock", "dclock", "tag")


class Prog:
    def __init__(self):
        self.streams = {e: [] for e in ENGS}
        self.last_w = {}
        self.readers = {}
        self.clock = {e: {} for e in ENGS}
        self.dclock = {e: {} for e in ENGS}
        self.slot_count = {}
        self.slot_last = {}
        self.out_dmas = []
        self.last_acc = {}
        self.tag = ""
        self.pe_log = []

    def add(self, eng, fn, reads=(), writes=(), slot=None, is_out=False):
        excl = [k for k in list(reads) + list(writes) if isinstance(k, tuple) and k[0] == "ps"]
        op = Op()
        op.eng, op.fn = eng, fn
        op.tag = self.tag
        st = self.streams[eng]
        op.idx = len(st)
        op.signal = False
        op.tick = None
        op.slot = slot
        op.waits = []
        deps = []
        seen = set()

        def push(d):
            if d is not None and id(d) not in seen:
                seen.add(id(d))
                deps.append(d)

        for k in reads:
            push(self.last_w.get(k))
        for k in writes:
            push(self.last_w.get(k))
            for r in self.readers.get(k, ()):
                push(r)
        if slot is not None:
            push(self.slot_last.get(slot))
        for k in excl:
            for e2, d in self.last_acc.setdefault(k, {}).items():
                if e2 != eng:
                    push(d)
        ck = self.clock[eng]
        dk = self.dclock[eng]
        for d in deps:
            if d.slot is not None:
                if dk.get(d.slot, 0) >= d.count:
                    continue
                op.waits.append(d)
                dk[d.slot] = d.count
            else:
                if d.eng == eng and eng == "pe":
                    continue
                if ck.get(d.eng, 0) >= d.idx + 1:
                    continue
                op.waits.append(d)
                d.signal = True
                ck[d.eng] = d.idx + 1
            for e2, v in d.clock.items():
                if ck.get(e2, 0) < v:
                    ck[e2] = v
            for s2, v in d.dclock.items():
                if dk.get(s2, 0) < v:
                    dk[s2] = v
        if slot is not None:
            c = self.slot_count.get(slot, 0) + 1
            self.slot_count[slot] = c
            op.count = c
            self.slot_last[slot] = op
            op.clock = {e: v for e, v in ck.items() if e != eng}
            op.dclock = dict(dk)
            if is_out:
                self.out_dmas.append(op)
        else:
            op.count = 0
            op.clock = dict(ck)
            op.dclock = dict(dk)
        for k in excl:
            self.last_acc[k][eng] = op
        for k in reads:
            self.readers.setdefault(k, []).append(op)
        for k in writes:
            self.last_w[k] = op
            self.readers[k] = []
        st.append(op)
        return op

    def emit(self, nc):
        nsig = {}
        for e in ENGS:
            n = 0
            for op in self.streams[e]:
                if op.slot is None and op.signal:
                    n += 1
                    op.tick = n
            nsig[e] = n
        with ExitStack() as es:
            esem = {}
            for e in ENGS:
                k = max(1, (nsig[e] + SEM_CAP - 1) // SEM_CAP)
                esem[e] = [es.enter_context(nc.semaphore(f"s_{e}_{i}")) for i in range(k)]
            ssem = {}
            for i, s in enumerate(self.slot_count):
                ssem[s] = es.enter_context(nc.semaphore(f"d_{i}"))
            block = es.enter_context(nc.Block())

            def run(e, eng):
                for op in self.streams[e]:
                    for d in op.waits:
                        if d.slot is not None:
                            eng.wait_ge(ssem[d.slot], 16 * d.count)
                        else:
                            t = d.tick - 1
                            eng.wait_ge(esem[d.eng][t // SEM_CAP], t % SEM_CAP + 1)
                    if e == "pe" and self.pe_log is not None:
                        cnt = [0]

                        class _Px:
                            def matmul(_s, *a, **k):
                                cnt[0] += 1
                                return eng.matmul(*a, **k)

                            def transpose(_s, *a, **k):
                                cnt[0] += 1
                                return eng.transpose(*a, **k)
                        ins = op.fn(_Px())
                        self.pe_log.append((op.tag, cnt[0]))
                    else:
                        ins = op.fn(eng)
                    if op.slot is not None:
                        ins.then_inc(ssem[op.slot], 16)
                    elif op.signal:
                        t = op.tick - 1
                        ins.then_inc(esem[e][t // SEM_CAP], 1)
                if e == "sp":
                    last = {}
                    for d in self.out_dmas:
                        last[d.slot] = max(last.get(d.slot, 0), d.count)
                    for s, c in last.items():
                        eng.wait_ge(ssem[s], 16 * c)

            @block.tensor
            def _(eng):
                run("pe", eng)

            @block.scalar
            def _(eng):
                run("act", eng)

            @block.vector
            def _(eng):
                run("dve", eng)

            @block.gpsimd
            def _(eng):
                run("pool", eng)

            @block.sync
            def _(eng):
                run("sp", eng)
        return nsig


D = 1024
DFF = 2816
NFF = 22
MT = 1024
NBLK = 8
EPS = 1e-6
O_AQ, O_AK, O_AV, O_RQ, O_RK, O_RV, O_RG, O_GA, O_GR = 0, 512, 640, 768, 1280, 1792, 2816, 3840, 4864
NCH = 29
C_ID, C_ONE, C_DEC, C_DK, C_DQ, C32_N = 0, 128, 256, 768, 1280, 1792
B_ONESN, B_ONES256, B_OPAD, B_E, B_ID, CBF_N = 0, 128, 256, 512, 2560, 2688


def make_consts():
    c32 = np.zeros((128, C32_N), np.float32)
    c32[:, C_ID:C_ID + 128] = np.eye(128, dtype=np.float32)
    c32[:, C_ONE:C_ONE + 128] = 1.0
    log_g = np.log(1.0 - np.exp2(-5.0 - np.arange(4, dtype=np.float64)))
    j = np.arange(128)[:, None].astype(np.float64)
    i = np.arange(128)[None, :].astype(np.float64)
    for h in range(4):
        dec = np.where(i >= j, np.exp(log_g[h] * np.maximum(i - j, 0.0)), 0.0) * (128.0 ** -0.5)
        c32[:, C_DEC + h * 128:C_DEC + (h + 1) * 128] = dec
        c32[:, C_DK + h * 128:C_DK + (h + 1) * 128] = (np.exp(log_g[h] * (127.0 - j)) * (128.0 ** -0.5))
        c32[:, C_DQ + h * 128:C_DQ + (h + 1) * 128] = np.exp(log_g[h] * (i + 1.0))
    gC = [float(np.exp(log_g[h] * 128.0)) for h in range(4)]
    cbf = np.zeros((128, CBF_N), np.float32)
    cbf[:, B_ONESN:B_ONESN + 128] = 1.0 / 1024
    cbf[:, B_ONES256:B_ONES256 + 128] = 1.0 / 256
    cbf[:, B_OPAD:B_OPAD + 64] = 1.0
    cbf[:, B_OPAD + 128 + 64:B_OPAD + 256] = 1.0
    cbf[:, B_ID:B_ID + 128] = np.eye(128, dtype=np.float32)
    slopes = np.exp2(-8.0 * (np.arange(8, dtype=np.float64) + 1.0) / 8)
    for g in range(2):
        for half in range(2):
            for p in range(2):
                for cc in range(2):
                    hd = 4 * g + 2 * cc + p
                    dist = (i + 128.0 - j) if half == 0 else (i - j)
                    val = np.where((dist >= 0) & (dist < 128), np.exp(-slopes[hd] * dist), 0.0)
                    o = B_E + g * 1024 + half * 512 + p * 256 + cc * 128
                    cbf[:, o:o + 128] = val
    return c32, cbf, gC


def prm_layout(depth):
    return 28 * depth + 8


def build(nseq, seq, depth):
    assert seq % MT == 0
    mt_per_seq = seq // MT
    nc = bass.Bass("TRN2", target_bir_lowering=False)
    x_d = nc.dram_tensor("x", [nseq, seq, D], F32, kind="ExternalInput").ap()
    w_in = nc.dram_tensor("w_in", [depth, D, 5888], F32, kind="ExternalInput").ap()
    w_ao = nc.dram_tensor("w_att_o", [depth, 512, D], F32, kind="ExternalInput").ap()
    w_ro = nc.dram_tensor("w_ret_o", [depth, D, D], F32, kind="ExternalInput").ap()
    w_o = nc.dram_tensor("w_out", [depth, D, D], F32, kind="ExternalInput").ap()
    w_g = nc.dram_tensor("w_gate", [depth, D, DFF], F32, kind="ExternalInput").ap()
    w_u = nc.dram_tensor("w_up", [depth, D, DFF], F32, kind="ExternalInput").ap()
    w_d = nc.dram_tensor("w_down", [depth, DFF, D], F32, kind="ExternalInput").ap()
    c32_d = nc.dram_tensor("c32", [128, C32_N], F32, kind="ExternalInput").ap()
    cbf_d = nc.dram_tensor("cbf", [128, CBF_N], F32, kind="ExternalInput").ap()
    NP = prm_layout(depth)
    prm_d = nc.dram_tensor("prm", [128, NP], F32, kind="ExternalInput").ap()
    y_d = nc.dram_tensor("y", [nseq, seq, D], F32, kind="ExternalOutput").ap()
    _, _, gC = make_consts()

    P = Prog()
    with ExitStack() as es:
        def sb(name, shape, dt):
            return es.enter_context(nc.sbuf_tensor(name, shape, dt))

        xT = sb("xT", [128, 8, MT], F32)
        hT = sb("hT", [128, 8, MT], BF16)
        A = sb("arena", [128, NCH * 1024], BF16)
        NBUF = 5
        wbuf = [sb(f"wb{i}", [128, 4096], BF16) for i in range(NBUF)]
        c32 = sb("c32s", [128, C32_N], F32)
        cbf = sb("cbfs", [128, CBF_N], BF16)
        prm = sb("prms", [128, NP], F32)
        esink = sb("esink", [128, 4 * depth], F32)
        epsb = sb("epsb", [128, 1], F32)
        vpad = sb("vpad", [128, 9 * 512], BF16)
        vcarry = [sb(f"vcar{l}", [128, 512], BF16) for l in range(depth)]
        akcarry = [sb(f"akcar{l}", [128, 4, 128], BF16) for l in range(depth)]
        state = [sb(f"state{l}", [128, 1024], F32) for l in range(depth)]
        state_bf = sb("state_bf", [128, 1024], BF16)
        stg = [A[:, i * 2048:(i + 1) * 2048].bitcast(F32) for i in range(2)]
        sqt = [sb(f"sqt{i}", [128, 512], BF16) for i in range(2)]
        rinv = sb("rinv", [128, 512], F32)
        etmp2 = sb("etmp2", [128, 1024], F32)
        gtmp = [etmp2[:, i * 512:(i + 1) * 512] for i in range(2)]
        ST2 = [sb(f"ST{i}", [128, 512], BF16) for i in range(2)]
        qd2 = [sb(f"qdT{i}", [128, 512], BF16) for i in range(2)]
        etmp = sb("etmp", [128, 1024], F32)
        pT = [sb(f"pT{i}", [128, 1024], BF16) for i in range(2)]
        den = sb("den", [128, 512], F32)
        osq = [sb(f"osq{i}", [128, 512], BF16) for i in range(2)]
        pt = [es.enter_context(nc.psum_tensor(f"pt{i}", [128, 1024], F32)) for i in range(4)]

        def bank(i):
            return pt[i // 2][:, (i % 2) * 512:(i % 2) * 512 + 512]

        def bk(i):
            return ("ps", i)

        bank_ctr = [0]

        def nextbank():
            b = bank_ctr[0] % 8
            bank_ctr[0] += 1
            return b

        def AR(ch, lo=0, hi=1024):
            return A[:, ch * 1024 + lo:ch * 1024 + hi]

        def akeys(lo, hi):
            return [("ar", g) for g in range(lo // 512, (hi - 1) // 512 + 1)]

        def ark(ch, lo=0, hi=1024):
            return akeys(ch * 1024 + lo, ch * 1024 + hi)

        def AR3(ch0, n, lo, hi):
            return A[:, ch0 * 1024:(ch0 + n) * 1024].rearrange("p (c t) -> p c t", c=n)[:, :, lo:hi]

        def ark3(ch0, n, lo, hi):
            ks = []
            for c in range(n):
                ks += ark(ch0 + c, lo, hi)
            return ks

        KPB = 8 * 1024

        def KP(q, lo, hi):
            return A[:, KPB + q * 1152 + lo:KPB + q * 1152 + hi]

        def kpk(q, lo, hi):
            return akeys(KPB + q * 1152 + lo, KPB + q * 1152 + hi)

        def tts(tt):
            return slice(tt * 512, tt * 512 + 512)

        eng_rr = [0]

        def evac_eng():
            eng_rr[0] += 1
            return "act" if eng_rr[0] % 2 else "dve"

        def copy_op(eng, out, in_, reads, writes, scale=None):
            if eng == "act":
                if scale is None:
                    P.add("act", lambda e: e.activation(out=out, in_=in_, func=AF.Copy), reads=reads, writes=writes)
                else:
                    P.add("act", lambda e: e.activation(out=out, in_=in_, func=AF.Copy, scale=scale),
                          reads=reads, writes=writes)
            else:
                assert scale is None
                P.add("dve", lambda e: e.tensor_copy(out=out, in_=in_), reads=reads, writes=writes)

        def mm_group(out_ap, pairs, reads, bkeys):
            def f(e):
                n = len(pairs)
                ins = None
                for i, (l, r) in enumerate(pairs):
                    ins = e.matmul(out_ap, l, r, start=(i == 0), stop=(i == n - 1))
                return ins
            P.add("pe", f, reads=reads, writes=bkeys)

        def mm_multi(groups, reads, bkeys):
            def f(e):
                ins = None
                for out_ap, pairs in groups:
                    n = len(pairs)
                    for i, (l, r) in enumerate(pairs):
                        ins = e.matmul(out_ap, l, r, start=(i == 0), stop=(i == n - 1))
                return ins
            P.add("pe", f, reads=reads, writes=bkeys)

        hkeys = {tt: [("hT", c, tt) for c in range(8)] for tt in range(2)}

        P.add("sp", lambda e: e.dma_start(out=c32[:, :], in_=c32_d[:, :]), writes=["c32"], slot="c32")
        P.add("sp", lambda e: e.dma_start(out=prm[:, :], in_=prm_d[:, :]), writes=["prm"], slot="prm")
        P.add("pool", lambda e: e.dma_start(out=cbf[:, :], in_=cbf_d[:, :]), writes=["cbf"], slot="cbf")
        P.add("dve", lambda e: e.memset(epsb[:, :], EPS), writes=["epsb"])
        P.add("dve", lambda e: e.memset(vpad[:, :], 0.0), writes=["vpad"])
        for l in range(depth):
            P.add("act", lambda e, l=l: e.activation(out=esink[:, 4 * l:4 * l + 4],
                                                     in_=prm[:, 28 * l + 24:28 * l + 28], func=AF.Exp),
                  reads=["prm"], writes=[("esink", l)])

        units = []

        def unit_simple(src, KC, ncols):
            return [(0, KC, ncols, 0, ncols, src.rearrange("(k p) n -> p k n", p=128))]

        items = []

        loaded = [0]
        all_units = []

        def emit_load(ui):
            pieces = all_units[ui]
            j = ui % NBUF
            for pi, (dlo, KC, ncols, clo, cn, src) in enumerate(pieces):
                dst = wbuf[j][:, dlo:dlo + KC * ncols].rearrange("p (k n) -> p k n", k=KC)[:, :, clo:clo + cn]
                P.add("pool", lambda e, dst=dst, src=src: e.dma_start(out=dst, in_=src),
                      writes=[("w", j)], slot=("w", j, pi))

        def run_items():
            base = 0
            for us, _ in items:
                all_units.extend(us)
            ui = 0
            for us, fn in items:
                need = min(len(all_units), ui + NBUF)
                while loaded[0] < need:
                    emit_load(loaded[0])
                    loaded[0] += 1
                views = []
                for k, u in enumerate(us):
                    j = (ui + k) % NBUF
                    _, KC, ncols, _, _, _ = u[0]
                    views.append((wbuf[j][:, 0:KC * ncols].rearrange("p (k n) -> p k n", k=KC), ("w", j)))
                P.tag = getattr(fn, "__name__", "?")
                fn(views)
                ui += len(us)

        def norm_stage(gcol, final=False):
            def f_norm(_):
                for tt in range(2):
                    b = nextbank()
                    for c in range(8):
                        s = sqt[c % 2]
                        P.add("act", lambda e, s=s, c=c, tt=tt: e.activation(out=s[:, :], in_=xT[:, c, tts(tt)],
                                                                             func=AF.Square),
                              reads=[("xT", c, tt)], writes=[("sqt", c % 2)])
                        P.add("pe", lambda e, s=s, c=c, b=b: e.matmul(bank(b), cbf[:, B_ONESN:B_ONESN + 128], s[:, :],
                                                                      start=(c == 0), stop=(c == 7)),
                              reads=[("sqt", c % 2), "cbf"], writes=[bk(b)])
                    rb, rbk = (rinv, "rinv") if tt == 0 else (den, "den")
                    P.add("act", lambda e, b=b, rb=rb: e.activation(out=rb[:, :], in_=bank(b), func=AF.Ln,
                                                                    bias=epsb[:, 0:1]),
                          reads=[bk(b), "epsb"], writes=[rbk])
                    P.add("act", lambda e, rb=rb: e.activation(out=rb[:, :], in_=rb[:, :], func=AF.Exp, scale=-0.5),
                          reads=[rbk], writes=[rbk])
                    for c in range(8):
                        if final:
                            P.add("dve", lambda e, c=c, tt=tt, rb=rb: e.scalar_tensor_tensor(
                                out=xT[:, c, tts(tt)], in0=xT[:, c, tts(tt)], scalar=prm[:, gcol + c:gcol + c + 1],
                                in1=rb[:, :], op0=ALU.mult, op1=ALU.mult),
                                reads=[("xT", c, tt), "prm", rbk], writes=[("xT", c, tt)])
                        else:
                            P.add("dve", lambda e, c=c, tt=tt, rb=rb: e.scalar_tensor_tensor(
                                out=hT[:, c, tts(tt)], in0=xT[:, c, tts(tt)], scalar=prm[:, gcol + c:gcol + c + 1],
                                in1=rb[:, :], op0=ALU.mult, op1=ALU.mult),
                                reads=[("xT", c, tt), "prm", rbk], writes=[("hT", c, tt)])
            items.append(([], f_norm))

        def fm_proj(wv, wk, ci, tt, KC, src, srckeys, b=None):
            if b is None:
                b = nextbank()
            mm_group(bank(b), [(wv[:, k, ci * 128:(ci + 1) * 128], src(k, tt)) for k in range(KC)],
                     reads=[wk] + srckeys, bkeys=[bk(b)])
            return b

        def h_src(k, tt):
            return hT[:, k, tts(tt)]

        def mixer_stage(l, first_mt, last_mt):
            wl = w_in[l]
            pb = 28 * l

            def f_rq(views):
                (wv, wk), = views
                for tt in range(2):
                    for h in range(4):
                        b = fm_proj(wv, wk, h, tt, 8, h_src, hkeys[tt])
                        copy_op(evac_eng(), AR(h, tt * 512, tt * 512 + 512), bank(b), [bk(b)], ark(h, tt * 512, tt * 512 + 512))
            items.append(([unit_simple(wl[:, O_RQ:O_RQ + 512], 8, 512)], f_rq))

            def f_rk(views):
                (wv, wk), = views
                for h in range(4):
                    for tt in range(2):
                        b = fm_proj(wv, wk, h, tt, 8, h_src, hkeys[tt])
                        copy_op(evac_eng(), AR(4 + h, tt * 512, tt * 512 + 512), bank(b), [bk(b)],
                                ark(4 + h, tt * 512, tt * 512 + 512))
            items.append(([unit_simple(wl[:, O_RK:O_RK + 512], 8, 512)], f_rk))

            def rk_tm_transposes():
                for blk in range(NBLK):
                    b = nextbank()
                    mm_multi([(bank(b)[:, h * 128:(h + 1) * 128],
                               [(AR(4 + h, blk * 128, blk * 128 + 128), cbf[:, B_ID:B_ID + 128])]) for h in range(4)],
                             reads=ark3(4, 4, blk * 128, blk * 128 + 128) + ["cbf"], bkeys=[bk(b)])
                    lo = (blk % 2) * 512
                    P.add("dve", lambda e, b=b, blk=blk, lo=lo: e.tensor_tensor(
                        out=AR(8 + blk // 2, lo, lo + 512), in0=bank(b), in1=c32[:, C_DK:C_DK + 512], op=ALU.mult),
                        reads=[bk(b), "c32"], writes=ark(8 + blk // 2, lo, lo + 512))

            for half in range(2):
                def f_rv(views, half=half):
                    (wv, wk), = views
                    for blk in range(NBLK):
                        b = nextbank()
                        tt = blk // 4
                        mm_group(bank(b), [(hT[:, k, blk * 128:(blk + 1) * 128], wv[:, k, :]) for k in range(8)],
                                 reads=[wk] + hkeys[tt], bkeys=[bk(b)])
                        copy_op(evac_eng(), AR(12 + blk, half * 512, half * 512 + 512), bank(b), [bk(b)],
                                ark(12 + blk, half * 512, half * 512 + 512))
                    if half == 1:
                        rk_tm_transposes()
                items.append(([unit_simple(wl[:, O_RV + half * 512:O_RV + half * 512 + 512], 8, 512)], f_rv))

            def f_ret(_):
                if not first_mt:
                    P.add("act", lambda e: e.activation(out=state_bf[:, :], in_=state[l][:, :], func=AF.Copy),
                          reads=[("state", l)], writes=["state_bf"])

                def isfirst(blk):
                    return first_mt and blk == 0

                def islast(blk):
                    return last_mt and blk == NBLK - 1

                def pe_front(blk):
                    sb_ = blk % 2
                    mm_multi([(bank(sb_)[:, h * 128:(h + 1) * 128], [(AR(4 + h, blk * 128, blk * 128 + 128),
                                                                      AR(h, blk * 128, blk * 128 + 128))]) for h in range(4)],
                             reads=ark3(0, 8, blk * 128, blk * 128 + 128), bkeys=[bk(sb_)])
                    if not islast(blk):
                        lo = (blk % 2) * 512
                        su = pt[2 + blk % 2]
                        mm_multi([(su[:, h * 256:(h + 1) * 256],
                                   [(AR(8 + blk // 2, lo + h * 128, lo + h * 128 + 128), AR(12 + blk, h * 256, h * 256 + 256))])
                                  for h in range(4)],
                                 reads=ark(8 + blk // 2, lo, lo + 512) + ark(12 + blk),
                                 bkeys=[bk(4 + 2 * (blk % 2)), bk(5 + 2 * (blk % 2))])

                def dve_front(blk):
                    sb_ = blk % 2
                    STb = ST2[blk % 2]
                    P.add("dve", lambda e: e.tensor_tensor(out=STb[:, :], in0=bank(sb_), in1=c32[:, C_DEC:C_DEC + 512],
                                                           op=ALU.mult),
                          reads=[bk(sb_), "c32"], writes=[("ST", blk % 2)])
                    if not isfirst(blk):
                        qb = qd2[blk % 2]
                        P.add("pool", lambda e: e.tensor_tensor(
                            out=qb[:, :].rearrange("p (h i) -> p h i", h=4), in0=AR3(0, 4, blk * 128, blk * 128 + 128),
                            in1=c32[:, C_DQ:C_DQ + 512].rearrange("p (h i) -> p h i", h=4), op=ALU.mult),
                            reads=ark3(0, 4, blk * 128, blk * 128 + 128) + ["c32"], writes=[("qdT", blk % 2)])

                def back(blk):
                    first = isfirst(blk)
                    STb = ST2[blk % 2]
                    qb = qd2[blk % 2]
                    groups = []
                    for h in range(4):
                        for jj in range(2):
                            c = 2 * h + jj
                            pairs = [(AR(12 + blk, h * 256 + jj * 128, h * 256 + jj * 128 + 128), STb[:, h * 128:(h + 1) * 128])]
                            if not first:
                                pairs.append((state_bf[:, h * 256 + jj * 128:h * 256 + jj * 128 + 128],
                                              qb[:, h * 128:(h + 1) * 128]))
                            groups.append((pt[1][:, c * 128:(c + 1) * 128], pairs))
                    mm_multi(groups, reads=ark(12 + blk) + [("ST", blk % 2)] + ([] if first else ["state_bf", ("qdT", blk % 2)]),
                             bkeys=[bk(2), bk(3)])
                    copy_op("act", AR3(21, 8, blk * 128, blk * 128 + 128), pt[1][:, :].rearrange("p (c t) -> p c t", c=8),
                            [bk(2), bk(3)], ark3(21, 8, blk * 128, blk * 128 + 128))
                    if not islast(blk):
                        su = pt[2 + blk % 2]
                        sk = [bk(4 + 2 * (blk % 2)), bk(5 + 2 * (blk % 2))]
                        if first:
                            P.add("dve", lambda e: e.tensor_copy(out=state[l][:, :], in_=su[:, :]),
                                  reads=sk, writes=[("state", l)])
                        else:
                            for h in range(4):
                                P.add("dve", lambda e, h=h: e.scalar_tensor_tensor(
                                    out=state[l][:, h * 256:(h + 1) * 256], in0=state[l][:, h * 256:(h + 1) * 256],
                                    scalar=gC[h], in1=su[:, h * 256:(h + 1) * 256], op0=ALU.mult, op1=ALU.add),
                                    reads=sk + [("state", l)], writes=[("state", l)])
                        P.add("act", lambda e: e.activation(out=state_bf[:, :], in_=state[l][:, :], func=AF.Copy),
                              reads=[("state", l)], writes=["state_bf"])

                pe_front(0)
                for blk in range(NBLK):
                    dve_front(blk)
                    if blk + 1 < NBLK:
                        pe_front(blk + 1)
                    back(blk)
            items.append(([], f_ret))

            akv_src = lambda c0, n: wl[:, c0:c0 + n].rearrange("(k p) n -> p k n", p=128)
            akv_unit = [(0, 8, 384, 0, 64, akv_src(O_AK, 64)), (0, 8, 384, 64, 64, akv_src(O_AK, 64)),
                        (0, 8, 384, 128, 64, akv_src(O_AK + 64, 64)), (0, 8, 384, 192, 64, akv_src(O_AK + 64, 64)),
                        (0, 8, 384, 256, 128, akv_src(O_AV, 128))]

            def f_gn(views):
                (wq, wqk), (wkv, wkvk) = views
                fillers = []
                for c in range(4):
                    for tt in range(2):
                        def f_aq_tile(c=c, tt=tt):
                            lo, hi = tt * 512, tt * 512 + 512
                            b = fm_proj(wq, wqk, c, tt, 8, h_src, hkeys[tt])
                            return lambda: copy_op(evac_eng(), AR(c, lo, hi), bank(b), [bk(b)], ark(c, lo, hi))
                        fillers.append(f_aq_tile)

                def akv_prep():
                    for q in range(4):
                        p = q % 2
                        rows = slice(64, 128) if p == 0 else slice(0, 64)
                        P.add("dve", lambda e, q=q, rows=rows: e.memset(A[rows, KPB + q * 1152:KPB + (q + 1) * 1152], 0.0),
                              writes=kpk(q, 0, 1152))
                    if not first_mt:
                        P.add("dve", lambda e: e.tensor_copy(
                            out=A[:, KPB:KPB + 4 * 1152].rearrange("p (q t) -> p q t", q=4)[:, :, 0:128], in_=akcarry[l][:, :, :]),
                            reads=[("akcar", l)], writes=[k for q in range(4) for k in kpk(q, 0, 128)])
                        P.add("dve", lambda e: e.tensor_copy(out=vpad[:, 0:512], in_=vcarry[l][:, :]),
                              reads=[("vcar", l)], writes=["vpad"])

                for g in range(2):
                    for tt in range(2):
                        def f_k_tile(g=g, tt=tt):
                            b = fm_proj(wkv, wkvk, g, tt, 8, h_src, hkeys[tt])
                            lo = 128 + tt * 512

                            def ev():
                                P.add("act", lambda e: e.activation(out=A[0:64, KPB + (2 * g) * 1152 + lo:KPB + (2 * g) * 1152 + lo + 512],
                                                                    in_=bank(b)[0:64, :], func=AF.Copy),
                                      reads=[bk(b)], writes=kpk(2 * g, lo, lo + 512))
                                P.add("dve", lambda e: e.tensor_copy(
                                    out=A[64:128, KPB + (2 * g + 1) * 1152 + lo:KPB + (2 * g + 1) * 1152 + lo + 512], in_=bank(b)[64:128, :]),
                                    reads=[bk(b)], writes=kpk(2 * g + 1, lo, lo + 512))
                            return ev
                        fillers.append(f_k_tile)
                for bq in range(2):
                    def f_v_tile(bq=bq):
                        b = nextbank()
                        groups = []
                        for bi in range(4):
                            blk = bq * 4 + bi
                            groups.append((bank(b)[:, bi * 128:(bi + 1) * 128],
                                           [(hT[:, k, blk * 128:(blk + 1) * 128], wkv[:, k, 256:384]) for k in range(8)]))
                        mm_multi(groups, reads=[wkvk] + hkeys[bq], bkeys=[bk(b)])

                        def ev():
                            vv = vpad[:, :].rearrange("p (s q d) -> p s q d", s=9, q=4)
                            src4 = bank(b).rearrange("p (s d) -> p s d", s=4)
                            for g in range(2):
                                for p in range(2):
                                    dst = vv[:, 1 + bq * 4:1 + bq * 4 + 4, 2 * g + p, 64 * p:64 * p + 64]
                                    srcv = src4[:, :, g * 64:(g + 1) * 64]
                                    copy_op(evac_eng(), dst, srcv, [bk(b)], ["vpad"])
                        return ev
                    fillers.append(f_v_tile)

                pending = []

                def fill(n):
                    for _ in range(n):
                        if fillers:
                            pending.append(fillers.pop(0)())

                def drain():
                    while pending:
                        pending.pop(0)()

                akv_prep()
                slot = 0
                for h in range(4):
                    for tt in range(2):
                        lo, hi = tt * 512, tt * 512 + 512
                        bm, bq = nextbank(), nextbank()
                        for jj in range(2):
                            c = 21 + 2 * h + jj
                            P.add("act", lambda e, c=c, jj=jj, lo=lo, hi=hi: e.activation(out=osq[jj][:, :], in_=AR(c, lo, hi),
                                                                                          func=AF.Square),
                                  reads=ark(c, lo, hi), writes=[("osq", jj)])
                        mm_group(bank(bm), [(cbf[:, B_ONES256:B_ONES256 + 128], AR(21 + 2 * h + jj, lo, hi)) for jj in range(2)],
                                 reads=["cbf"] + ark(21 + 2 * h, lo, hi) + ark(22 + 2 * h, lo, hi), bkeys=[bk(bm)])
                        mm_group(bank(bq), [(cbf[:, B_ONES256:B_ONES256 + 128], osq[jj][:, :]) for jj in range(2)],
                                 reads=["cbf", ("osq", 0), ("osq", 1)], bkeys=[bk(bq)])
                        fill(2 if slot % 4 != 3 else 1)
                        slot += 1
                        P.add("act", lambda e, bm=bm: e.activation(out=etmp[:, 0:512], in_=bank(bm), func=AF.Square),
                              reads=[bk(bm)], writes=[("etmp", 0)])
                        P.add("dve", lambda e, bq=bq: e.tensor_tensor(out=etmp[:, 512:1024], in0=bank(bq), in1=etmp[:, 0:512],
                                                                      op=ALU.subtract),
                              reads=[bk(bq), ("etmp", 0)], writes=[("etmp", 1)])
                        P.add("act", lambda e: e.activation(out=etmp[:, 512:1024], in_=etmp[:, 512:1024], func=AF.Ln,
                                                            bias=epsb[:, 0:1]),
                              reads=[("etmp", 1), "epsb"], writes=[("etmp", 1)])
                        P.add("act", lambda e: e.activation(out=etmp[:, 512:1024], in_=etmp[:, 512:1024], func=AF.Exp,
                                                            scale=-0.5),
                              reads=[("etmp", 1)], writes=[("etmp", 1)])
                        for jj in range(2):
                            c = 21 + 2 * h + jj
                            gcol = pb + 16 + 2 * h + jj
                            gt = gtmp[jj]
                            P.add("dve", lambda e, c=c, gt=gt, bm=bm, lo=lo, hi=hi: e.tensor_tensor(
                                out=gt[:, :], in0=AR(c, lo, hi), in1=bank(bm), op=ALU.subtract),
                                reads=ark(c, lo, hi) + [bk(bm)], writes=[("gtmp", jj)])
                            P.add("dve", lambda e, c=c, gt=gt, gcol=gcol, lo=lo, hi=hi: e.scalar_tensor_tensor(
                                out=AR(c, lo, hi), in0=gt[:, :], scalar=prm[:, gcol:gcol + 1], in1=etmp[:, 512:1024],
                                op0=ALU.mult, op1=ALU.mult),
                                reads=[("gtmp", jj), "prm", ("etmp", 1)], writes=ark(c, lo, hi))
                        drain()
                while fillers:
                    fill(1)
                    drain()
            items.append(([unit_simple(wl[:, O_AQ:O_AQ + 512], 8, 512), akv_unit], f_gn))

            for u in range(2):
                def f_rg(views, u=u):
                    (wv, wk), = views
                    for ci in range(4):
                        c = u * 4 + ci
                        for tt in range(2):
                            lo, hi = tt * 512, tt * 512 + 512
                            b = fm_proj(wv, wk, ci, tt, 8, h_src, hkeys[tt])
                            gi = (c * 2 + tt) % 2
                            gt = gtmp[gi]
                            P.add("act", lambda e, b=b, gt=gt: e.activation(out=gt[:, :], in_=bank(b), func=AF.Silu),
                                  reads=[bk(b)], writes=[("gtmp", gi)])
                            P.add("dve", lambda e, c=c, gt=gt, lo=lo, hi=hi: e.tensor_tensor(
                                out=AR(21 + c, lo, hi), in0=AR(21 + c, lo, hi), in1=gt[:, :], op=ALU.mult),
                                reads=ark(21 + c, lo, hi) + [("gtmp", gi)], writes=ark(21 + c, lo, hi))
                items.append(([unit_simple(wl[:, O_RG + u * 512:O_RG + u * 512 + 512], 8, 512)], f_rg))

            def f_att(_):
                vv = vpad[:, :].rearrange("p (s q d) -> p s q d", s=9, q=4)

                def halves_of(blk):
                    return [1] if (first_mt and blk == 0) else [0, 1]

                def S(blk, g):
                    groups = []
                    rk_ = []
                    for half in halves_of(blk):
                        for p in range(2):
                            q = 2 * g + p
                            kc = (blk + half) * 128
                            o = half * 512 + p * 256
                            groups.append((pt[g][:, o:o + 256],
                                           [(KP(q, kc, kc + 128), AR3(2 * g, 2, blk * 128, blk * 128 + 128))]))
                            rk_ += kpk(q, kc, kc + 128)
                    mm_multi(groups, reads=rk_ + ark3(0, 4, blk * 128, blk * 128 + 128), bkeys=[bk(2 * g), bk(2 * g + 1)])

                def softmax(blk, g):
                    lo = 512 if (first_mt and blk == 0) else 0
                    eb = etmp if g == 0 else etmp2
                    ek = [("etmp", 0), ("etmp", 1)] if g == 0 else [("gtmp", 0), ("gtmp", 1)]
                    P.add("act", lambda e: e.activation(out=eb[:, lo:1024], in_=pt[g][:, lo:1024], func=AF.Exp, scale=0.125),
                          reads=[bk(2 * g), bk(2 * g + 1)], writes=ek)
                    pg = pT[g]
                    P.add("dve", lambda e: e.tensor_tensor(
                        out=pg[:, lo:1024], in0=eb[:, lo:1024], in1=cbf[:, B_E + g * 1024 + lo:B_E + g * 1024 + 1024],
                        op=ALU.mult),
                        reads=ek + ["cbf"], writes=[("pT", g)])

                def PV(blk, g):
                    pg = pT[g]
                    ob = pt[2 + blk % 2]
                    pv, dn = [], []
                    for half in halves_of(blk):
                        for p in range(2):
                            o = half * 512 + p * 256
                            pv.append((vv[:, blk + half, 2 * g + p, :], pg[:, o:o + 256]))
                            dn.append((cbf[:, B_OPAD + p * 128:B_OPAD + p * 128 + 128], pg[:, o:o + 256]))
                    mm_multi([(ob[:, g * 256:g * 256 + 256], pv), (ob[:, 512 + g * 256:512 + g * 256 + 256], dn)],
                             reads=["vpad", "cbf", ("pT", g)], bkeys=[bk(4 + 2 * (blk % 2)), bk(5 + 2 * (blk % 2))])

                def fin(blk):
                    ob = pt[2 + blk % 2]
                    kb = [bk(4 + 2 * (blk % 2)), bk(5 + 2 * (blk % 2))]
                    for c in range(4):
                        P.add("act", lambda e, c=c: e.activation(
                            out=den[:, c * 128:(c + 1) * 128], in_=ob[:, 512 + c * 128:512 + (c + 1) * 128], func=AF.Ln,
                            bias=esink[:, 4 * l + c:4 * l + c + 1]),
                            reads=kb + [("esink", l)], writes=["den"])
                    P.add("act", lambda e: e.activation(out=den[:, :], in_=den[:, :], func=AF.Exp, scale=-1.0),
                          reads=["den"], writes=["den"])
                    P.add("dve", lambda e: e.tensor_tensor(
                        out=AR3(4, 4, blk * 128, blk * 128 + 128), in0=ob[:, 0:512].rearrange("p (c t) -> p c t", c=4),
                        in1=den[:, :].rearrange("p (c t) -> p c t", c=4), op=ALU.mult),
                        reads=kb + ["den"], writes=ark3(4, 4, blk * 128, blk * 128 + 128))

                S(0, 0)
                S(0, 1)
                softmax(0, 0)
                softmax(0, 1)
                for blk in range(NBLK):
                    PV(blk, 0)
                    if blk + 1 < NBLK:
                        S(blk + 1, 0)
                    PV(blk, 1)
                    if blk + 1 < NBLK:
                        S(blk + 1, 1)
                        softmax(blk + 1, 0)
                        softmax(blk + 1, 1)
                    fin(blk)
                if not last_mt:
                    P.add("dve", lambda e: e.tensor_copy(
                        out=akcarry[l][:, :, :], in_=A[:, KPB:KPB + 4 * 1152].rearrange("p (q t) -> p q t", q=4)[:, :, 1024:1152]),
                        reads=[k for q in range(4) for k in kpk(q, 1024, 1152)], writes=[("akcar", l)])
                    P.add("dve", lambda e: e.tensor_copy(out=vcarry[l][:, :], in_=vpad[:, 8 * 512:9 * 512]),
                          reads=["vpad"], writes=[("vcar", l)])
            items.append(([], f_att))

            for u in range(2):
                def f_ao(views, u=u):
                    (wa, wak), (wg, wgk) = views
                    for ci in range(4):
                        c = u * 4 + ci
                        for tt in range(2):
                            lo, hi = tt * 512, tt * 512 + 512
                            ba = fm_proj(wa, wak, ci, tt, 4, lambda k, tt: AR(4 + k, tt * 512, tt * 512 + 512),
                                         ark3(4, 4, lo, hi))
                            bg = fm_proj(wg, wgk, ci, tt, 8, h_src, hkeys[tt])
                            gi = (c * 2 + tt) % 2
                            gt = gtmp[gi]
                            P.add("act", lambda e, bg=bg, gt=gt: e.activation(out=gt[:, :], in_=bank(bg), func=AF.Sigmoid),
                                  reads=[bk(bg)], writes=[("gtmp", gi)])
                            P.add("dve", lambda e, c=c, ba=ba, gt=gt, lo=lo, hi=hi: e.tensor_tensor(
                                out=AR(13 + c, lo, hi), in0=bank(ba), in1=gt[:, :], op=ALU.mult),
                                reads=[bk(ba), ("gtmp", gi)], writes=ark(13 + c, lo, hi))
                items.append(([unit_simple(w_ao[l][:, u * 512:u * 512 + 512], 4, 512),
                               unit_simple(wl[:, O_GA + u * 512:O_GA + u * 512 + 512], 8, 512)], f_ao))

            for u in range(2):
                def f_ro(views, u=u):
                    (wr, wrk), (wg, wgk) = views
                    for ci in range(4):
                        c = u * 4 + ci
                        for tt in range(2):
                            lo, hi = tt * 512, tt * 512 + 512
                            br = fm_proj(wr, wrk, ci, tt, 8, lambda k, tt: AR(21 + k, tt * 512, tt * 512 + 512),
                                         ark3(21, 8, lo, hi))
                            bg = fm_proj(wg, wgk, ci, tt, 8, h_src, hkeys[tt])
                            gi = (c * 2 + tt) % 2
                            gt = gtmp[gi]
                            P.add("act", lambda e, bg=bg, gt=gt: e.activation(out=gt[:, :], in_=bank(bg), func=AF.Sigmoid),
                                  reads=[bk(bg)], writes=[("gtmp", gi)])
                            P.add("dve", lambda e, br=br, gt=gt: e.tensor_tensor(
                                out=gt[:, :], in0=bank(br), in1=gt[:, :], op=ALU.mult),
                                reads=[bk(br), ("gtmp", gi)], writes=[("gtmp", gi)])
                            P.add("dve", lambda e, c=c, gt=gt, lo=lo, hi=hi: e.tensor_tensor(
                                out=AR(13 + c, lo, hi), in0=AR(13 + c, lo, hi), in1=gt[:, :], op=ALU.add),
                                reads=ark(13 + c, lo, hi) + [("gtmp", gi)], writes=ark(13 + c, lo, hi))
                items.append(([unit_simple(w_ro[l][:, u * 512:u * 512 + 512], 8, 512),
                               unit_simple(wl[:, O_GR + u * 512:O_GR + u * 512 + 512], 8, 512)], f_ro))

            for u in range(2):
                def f_o(views, u=u):
                    (wv, wk), = views
                    for ci in range(4):
                        c = u * 4 + ci
                        for tt in range(2):
                            lo, hi = tt * 512, tt * 512 + 512
                            b = fm_proj(wv, wk, ci, tt, 8, lambda k, tt: AR(13 + k, tt * 512, tt * 512 + 512),
                                        ark3(13, 8, lo, hi))
                            P.add("dve", lambda e, c=c, b=b, tt=tt: e.tensor_tensor(
                                out=xT[:, c, tts(tt)], in0=xT[:, c, tts(tt)], in1=bank(b), op=ALU.add),
                                reads=[bk(b), ("xT", c, tt)], writes=[("xT", c, tt)])
                items.append(([unit_simple(w_o[l][:, u * 512:u * 512 + 512], 8, 512)], f_o))

        def ffn_stage(l):
            norm_stage(28 * l + 8)
            for u in range(6):
                n = 512 if u < 5 else 256

                def f_gu(views, u=u, n=n):
                    (wg, wgk), (wu, wuk) = views
                    for tt in range(2):
                        for ci in range(n // 128):
                            c = u * 4 + ci
                            lo, hi = tt * 512, tt * 512 + 512
                            bg = fm_proj(wg, wgk, ci, tt, 8, h_src, hkeys[tt])
                            bu = fm_proj(wu, wuk, ci, tt, 8, h_src, hkeys[tt])
                            gi = (ci + tt) % 2
                            gt = gtmp[gi]
                            P.add("act", lambda e, bg=bg, gt=gt: e.activation(out=gt[:, :], in_=bank(bg), func=AF.Silu),
                                  reads=[bk(bg)], writes=[("gtmp", gi)])
                            P.add("dve", lambda e, c=c, bu=bu, gt=gt, lo=lo, hi=hi: e.tensor_tensor(
                                out=AR(c, lo, hi), in0=bank(bu), in1=gt[:, :], op=ALU.mult),
                                reads=[bk(bu), ("gtmp", gi)], writes=ark(c, lo, hi))
                items.append(([unit_simple(w_g[l][:, u * 512:u * 512 + n], 8, n),
                               unit_simple(w_u[l][:, u * 512:u * 512 + n], 8, n)], f_gu))
            for c in range(8):
                def f_dn(views, c=c):
                    (wv, wk), = views
                    for tt in range(2):
                        lo, hi = tt * 512, tt * 512 + 512
                        b = nextbank()
                        mm_group(bank(b), [(wv[:, k, :], AR(k, lo, hi)) for k in range(NFF)],
                                 reads=[wk] + ark3(0, NFF, lo, hi), bkeys=[bk(b)])
                        P.add("dve", lambda e, c=c, b=b, tt=tt: e.tensor_tensor(
                            out=xT[:, c, tts(tt)], in0=xT[:, c, tts(tt)], in1=bank(b), op=ALU.add),
                            reads=[bk(b), ("xT", c, tt)], writes=[("xT", c, tt)])
                items.append(([unit_simple(w_d[l][:, c * 128:(c + 1) * 128], NFF, 128)], f_dn))

        stgL = [A[:, (22 + 2 * i) * 1024:(24 + 2 * i) * 1024].bitcast(F32) for i in range(2)]

        def ld_dma(s, t0, blk):
            sg = stgL[blk % 2]
            P.add("sp", lambda e: e.dma_start(out=sg[:, :], in_=x_d[s, t0 + blk * 128:t0 + blk * 128 + 128, :]),
                  writes=ark3(22 + 2 * (blk % 2), 2, 0, 1024), slot=("ld", blk % 2))

        def ld_xpose(blk):
            sg = stgL[blk % 2]
            tt = blk // 4
            for hf in range(2):
                b = nextbank()

                def f(e, hf=hf, b=b):
                    ins = None
                    for jx in range(4):
                        c = hf * 4 + jx
                        ins = e.transpose(bank(b)[:, jx * 128:(jx + 1) * 128], sg[:, c * 128:(c + 1) * 128],
                                          c32[:, C_ID:C_ID + 128])
                    return ins
                P.add("pe", f, reads=ark3(22 + 2 * (blk % 2), 2, 0, 1024) + ["c32"], writes=[bk(b)])
                copy_op(evac_eng(), xT[:, hf * 4:hf * 4 + 4, blk * 128:blk * 128 + 128],
                        bank(b).rearrange("p (c t) -> p c t", c=4), [bk(b)],
                        [("xT", c, tt) for c in range(hf * 4, hf * 4 + 4)])

        def st_block(s, t0, blk):
            sg = stg[blk % 2]
            tt = blk // 4
            for hf in range(2):
                b = nextbank()

                def f(e, hf=hf, b=b):
                    ins = None
                    for jx in range(4):
                        c = hf * 4 + jx
                        ins = e.transpose(bank(b)[:, jx * 128:(jx + 1) * 128], xT[:, c, blk * 128:blk * 128 + 128],
                                          c32[:, C_ID:C_ID + 128])
                    return ins
                P.add("pe", f, reads=[("xT", c, tt) for c in range(hf * 4, hf * 4 + 4)] + ["c32"], writes=[bk(b)])
                copy_op(evac_eng(), sg[:, hf * 512:hf * 512 + 512], bank(b), [bk(b)], ark(2 * (blk % 2) + hf))
            P.add("sp", lambda e: e.dma_start(out=y_d[s, t0 + blk * 128:t0 + blk * 128 + 128, :], in_=sg[:, :]),
                  reads=ark3(2 * (blk % 2), 2, 0, 1024), slot=("st", blk % 2), is_out=True)

        def load_stage(s, t0):
            def f_load(_):
                ld_dma(s, t0, 0)
                ld_dma(s, t0, 1)
                for blk in range(NBLK):
                    ld_xpose(blk)
                    if blk + 2 < NBLK:
                        ld_dma(s, t0, blk + 2)
            items.append(([], f_load))

        def store_load_stage(s, t0, gcol, nxt):
            if nxt is not None:
                def f_pre(_):
                    ld_dma(nxt[0], nxt[1], 0)
                    ld_dma(nxt[0], nxt[1], 1)
                items.append(([], f_pre))
            norm_stage(gcol, final=True)

            def f_store(_):
                for blk in range(4):
                    st_block(s, t0, blk)
                for blk in range(4):
                    if nxt is not None:
                        ld_xpose(blk)
                        ld_dma(nxt[0], nxt[1], blk + 2)
                    st_block(s, t0, 4 + blk)
                if nxt is not None:
                    for blk in range(4, NBLK):
                        ld_xpose(blk)
                        if blk + 2 < NBLK:
                            ld_dma(nxt[0], nxt[1], blk + 2)
            items.append(([], f_store))

        order = [(s, m * MT, m) for s in range(nseq) for m in range(mt_per_seq)]
        load_stage(order[0][0], order[0][1])
        for i, (s, t0, m) in enumerate(order):
            for l in range(depth):
                norm_stage(28 * l)
                mixer_stage(l, m == 0, m == mt_per_seq - 1)
                ffn_stage(l)
            nxt = (order[i + 1][0], order[i + 1][1]) if i + 1 < len(order) else None
            store_load_stage(s, t0, 28 * depth, nxt)
        run_items()
        nsig = P.emit(nc)
        global _LAST_PE_LOG
        _LAST_PE_LOG = P.pe_log
    return nc, nsig


def make_prm(norm_mix, norm_ffn, ret_gn_gain, att_sinks, final_norm, depth):
    NP = prm_layout(depth)
    prm = np.zeros((128, NP), np.float32)
    for l in range(depth):
        prm[:, 28 * l + 0:28 * l + 8] = np.asarray(norm_mix[l], np.float32).reshape(8, 128).T
        prm[:, 28 * l + 8:28 * l + 16] = np.asarray(norm_ffn[l], np.float32).reshape(8, 128).T
        prm[:, 28 * l + 16:28 * l + 24] = np.asarray(ret_gn_gain[l], np.float32).reshape(8, 128).T
        sk = np.asarray(att_sinks[l], np.float32).reshape(4, 2)
        prm[0:64, 28 * l + 24:28 * l + 28] = sk[:, 0][None, :]
        prm[64:128, 28 * l + 24:28 * l + 28] = sk[:, 1][None, :]
    prm[:, 28 * depth:28 * depth + 8] = np.asarray(final_norm, np.float32).reshape(8, 128).T
    return prm


_CACHE = {}
_LAST_PE_LOG = None


def run(x, norm_mix, w_in, att_sinks, ret_gn_gain, w_att_o, w_ret_o, w_out, norm_ffn, w_gate, w_up, w_down,
        final_norm, n_cores=8, runner=None):
    x = np.ascontiguousarray(np.asarray(x, np.float32))
    depth = int(np.asarray(w_in).shape[0])
    batch, seq, _ = x.shape
    nseq = batch // n_cores
    key = (nseq, seq, depth)
    if key not in _CACHE:
        _CACHE[key] = build(nseq, seq, depth)
    nc, _ = _CACHE[key]
    c32, cbf, _ = make_consts()
    prm = make_prm(norm_mix, norm_ffn, ret_gn_gain, att_sinks, final_norm, depth)
    shared = dict(
        w_in=np.ascontiguousarray(np.asarray(w_in, np.float32)),
        w_att_o=np.ascontiguousarray(np.asarray(w_att_o, np.float32)),
        w_ret_o=np.ascontiguousarray(np.asarray(w_ret_o, np.float32)),
        w_out=np.ascontiguousarray(np.asarray(w_out, np.float32)),
        w_gate=np.ascontiguousarray(np.asarray(w_gate, np.float32)),
        w_up=np.ascontiguousarray(np.asarray(w_up, np.float32)),
        w_down=np.ascontiguousarray(np.asarray(w_down, np.float32)),
        c32=c32, cbf=cbf, prm=prm)
    in_maps = []
    for i in range(n_cores):
        m = dict(shared)
        m["x"] = np.ascontiguousarray(x[i * nseq:(i + 1) * nseq])
        in_maps.append(m)
    if runner is None:
        res = run_bass_kernel_spmd(nc, in_maps, core_ids=list(range(n_cores))).results
    else:
        res = runner(nc, in_maps)
    return np.concatenate([np.asarray(r["y"], np.float32) for r in res], axis=0)


def kernel(x, norm_mix, w_in, att_sinks, ret_gn_gain, w_att_o, w_ret_o, w_out, norm_ffn, w_gate, w_up, w_down,
           final_norm):
    return run(x, norm_mix, w_in, att_sinks, ret_gn_gain, w_att_o, w_ret_o, w_out, norm_ffn, w_gate, w_up, w_down,
               final_norm, n_cores=8)
```
